# Optimizing a Trainium2 kernel written in Bass

```python
import jax
import jax.numpy as jnp
from jax import lax
import numpy as np

D_MODEL = 1024
BATCH = 2
SEQ = 8192
DEPTH = 1

CTX_LEN = 256
GRID_W = 64
MIX_W = D_MODEL
A_W = MIX_W // 2
A_GROUPS = 4
A_GROUP_DIM = A_W // A_GROUPS
A_CHUNK = 128
ROWS_PER_CHUNK = A_CHUNK // GRID_W
B_W = MIX_W - A_W
B_HEADS = 4
B_HEAD_DIM = B_W // B_HEADS
CONV_K = 3
GDN_CHUNK = 64
N_EXPERTS = 16
EC_CAPACITY = 2
EXPERT_FF = 1024
N_MOD = 6
NORM_EPS = 1e-6
COL_QKV = 3 * B_W
N_STATE_COLS = COL_QKV + 4 * B_HEADS
COL_Z_END = N_STATE_COLS + B_W
IN_COLS = COL_Z_END + 2 * A_W

kernel_name = "hybrid_gmlp_gdn_ec_dit_layer"


def rms_norm(x, g):
    xf = x.astype(jnp.float32)
    y = xf * lax.rsqrt(jnp.mean(xf * xf, axis=-1, keepdims=True) + NORM_EPS)
    return (y * g.astype(jnp.float32)).astype(x.dtype)


def l2_normalize(x):
    return x * lax.rsqrt(jnp.sum(x * x, axis=-1, keepdims=True) + NORM_EPS)


def short_conv(x, w):
    pad = (CONV_K - 1) // 2
    y = lax.conv_general_dilated(
        x, w[:, None, :].astype(x.dtype), window_strides=(1,), padding=[(pad, pad)],
        dimension_numbers=("NWC", "WIO", "NWC"), feature_group_count=x.shape[-1])
    return jax.nn.silu(y)


def chunk_mlp(uv, n_chunks, norm_g, ws, bs):
    B, T, _ = uv.shape
    u, v = jnp.split(jax.nn.gelu(uv, approximate=False), 2, axis=-1)
    v = rms_norm(v.reshape(B, T, A_GROUPS, A_GROUP_DIM), norm_g.reshape(A_GROUPS, A_GROUP_DIM))
    vc = v.reshape(B, n_chunks, A_CHUNK, A_GROUPS, A_GROUP_DIM)
    s = jnp.einsum("gts,bnsgd->bntgd", ws, vc) + jnp.swapaxes(bs, 0, 1)[None, None, :, :, None]
    return u * s.reshape(B, T, A_W)


def gdn_streams(p, conv_w, a_log, dt_bias):
    B, T, _ = p.shape
    qkv = short_conv(p[..., :COL_QKV], conv_w).astype(jnp.float32)
    qkv = qkv.reshape(B, T, 3, B_HEADS, B_HEAD_DIM)
    q = l2_normalize(qkv[:, :, 0]) * (B_HEAD_DIM ** -0.5)
    k = l2_normalize(qkv[:, :, 1])
    v = qkv[:, :, 2]
    ab = p[..., COL_QKV:N_STATE_COLS].astype(jnp.float32).reshape(B, T, 2, 2, B_HEADS)
    g = -jnp.exp(a_log.astype(jnp.float32)) * jax.nn.softplus(ab[:, :, 0] + dt_bias.astype(jnp.float32))
    beta = jax.nn.sigmoid(ab[:, :, 1])
    return q, k, v, g, beta


def gdn_chunked(q, k, v, g, beta, s0):
    B, T, H, Dk = q.shape
    C = GDN_CHUNK
    N = T // C

    def chunks(t):
        t = t.reshape(B, N, C, H, *t.shape[3:])
        return jnp.moveaxis(t, (1, 3), (0, 2))

    qc, kc, vc = chunks(q), chunks(k), chunks(v)
    gcum = jnp.cumsum(chunks(g), axis=-1)
    bc = chunks(beta)
    tri = jnp.tril(jnp.ones((C, C), dtype=bool))
    strict = jnp.tril(jnp.ones((C, C), dtype=bool), -1)
    diff = gcum[..., :, None] - gcum[..., None, :]
    decay = jnp.where(tri, jnp.exp(jnp.where(tri, diff, 0.0)), 0.0)
    kb = kc * bc[..., None]
    a_mat = jnp.where(strict, jnp.einsum("nbhid,nbhjd->nbhij", kb, kc) * decay, 0.0)
    eye = jnp.eye(C, dtype=jnp.float32)
    t_inv = lax.linalg.triangular_solve(eye + a_mat, jnp.broadcast_to(eye, a_mat.shape),
                                        left_side=True, lower=True, unit_diagonal=True)
    u = t_inv @ (vc * bc[..., None])
    w = t_inv @ (kb * jnp.exp(gcum)[..., None])
    attn = jnp.einsum("nbhid,nbhjd->nbhij", qc, kc) * decay
    q_g = qc * jnp.exp(gcum)[..., None]
    g_last = gcum[..., -1]
    k_g = kc * jnp.exp(g_last[..., None] - gcum)[..., None]

    def step(state, xs):
        q_i, k_i, u_i, w_i, a_i, gl_i = xs
        v_new = u_i - w_i @ state
        o_i = q_i @ state + a_i @ v_new
        state = state * jnp.exp(gl_i)[..., None, None] + jnp.swapaxes(k_i, -1, -2) @ v_new
        return state, o_i

    s_final, o = lax.scan(step, s0, (q_g, k_g, u, w, attn, g_last))
    o = jnp.moveaxis(o, (0, 2), (1, 3)).reshape(B, T, H, v.shape[-1])
    return o, s_final


def gdn_two_dirs(q, k, v, g, beta, s0_f, s0_b):
    o_f, s_f = gdn_chunked(q, k, v, g[:, :, 0], beta[:, :, 0], s0_f)
    flip = lambda t: jnp.flip(t, axis=1)
    o_b, s_b = gdn_chunked(flip(q), flip(k), flip(v), flip(g[:, :, 1]), flip(beta[:, :, 1]), s0_b)
    return o_f + flip(o_b), s_f, s_b


def gdn_out(o, z, norm_g):
    B, T = o.shape[:2]
    zh = z.reshape(B, T, B_HEADS, B_HEAD_DIM).astype(jnp.float32)
    y = o * lax.rsqrt(jnp.mean(o * o, axis=-1, keepdims=True) + NORM_EPS)
    y = y * norm_g.astype(jnp.float32) * jax.nn.silu(zh)
    return y.reshape(B, T, B_W)


def mix_out(p, o, n_chunks, gdn_norm_g, gm_norm_g, gm_ws, gm_bs, w_out):
    y_a = chunk_mlp(p[..., COL_Z_END:], n_chunks, gm_norm_g, gm_ws, gm_bs)
    y_b = gdn_out(o, p[..., N_STATE_COLS:COL_Z_END], gdn_norm_g).astype(p.dtype)
    return jnp.concatenate([y_a, y_b], axis=-1) @ w_out


def expert_choice_ffn(h, w_router, b_router, w_gate, w_up, w_down):
    B, N, D = h.shape
    cap = EC_CAPACITY * N // N_EXPERTS
    aff = jax.nn.softmax((h @ w_router + b_router).astype(jnp.float32), axis=-1)
    gate, idx = lax.top_k(jnp.swapaxes(aff, 1, 2), cap)
    xe = jax.vmap(lambda hb, ib: hb[ib])(h, idx)
    hid = jax.nn.silu(jnp.einsum("becd,edf->becf", xe, w_gate)) * jnp.einsum("becd,edf->becf", xe, w_up)
    ye = jnp.einsum("becf,efd->becd", hid, w_down) * gate[..., None].astype(h.dtype)
    return jax.vmap(lambda ib, yb: jnp.zeros((N, D), yb.dtype).at[ib.reshape(-1)].add(yb.reshape(-1, D)))(idx, ye)


def setup_inputs(seed: int = 0) -> dict:
    key = jax.random.key(seed)
    ks = jax.random.split(key, 24)
    f32 = jnp.float32
    nrm = lambda k, shape, s: jax.random.normal(k, shape, f32) * s
    dt = jnp.exp(jax.random.uniform(ks[10], (DEPTH, 2, B_HEADS), f32, np.log(1e-3), np.log(1e-1)))
    return {
        "x": nrm(ks[0], (BATCH, SEQ, D_MODEL), 1.0),
        "c": nrm(ks[1], (BATCH, D_MODEL), 1.0),
        "ctx": nrm(ks[2], (BATCH, CTX_LEN, D_MODEL), 1.0),
        "c_ctx": nrm(ks[3], (D_MODEL,), 1.0),
        "w_mod": nrm(ks[4], (DEPTH, D_MODEL, N_MOD * D_MODEL), 0.5 * D_MODEL ** -0.5),
        "b_mod": nrm(ks[5], (DEPTH, N_MOD * D_MODEL), 0.02),
        "norm1_g": 1.0 + nrm(ks[6], (DEPTH, D_MODEL), 0.05),
        "norm2_g": 1.0 + nrm(ks[7], (DEPTH, D_MODEL), 0.05),
        "w_in": nrm(ks[8], (DEPTH, D_MODEL, IN_COLS), D_MODEL ** -0.5),
        "conv_w": nrm(ks[9], (DEPTH, CONV_K, COL_QKV), CONV_K ** -0.5),
        "a_log": jnp.log(jax.random.uniform(ks[11], (DEPTH, 2, B_HEADS), f32, 1.0, 16.0)),
        "dt_bias": dt + jnp.log(-jnp.expm1(-dt)),
        "gdn_norm_g": 1.0 + nrm(ks[12], (DEPTH, B_HEAD_DIM), 0.05),
        "gm_norm_g": 1.0 + nrm(ks[13], (DEPTH, A_W), 0.05),
        "gm_ws": nrm(ks[14], (DEPTH, A_GROUPS, A_CHUNK, A_CHUNK), A_CHUNK ** -0.5),
        "gm_bs": 1.0 + nrm(ks[15], (DEPTH, A_GROUPS, A_CHUNK), 0.1),
        "w_out": nrm(ks[16], (DEPTH, MIX_W, D_MODEL), MIX_W ** -0.5),
        "w_router": nrm(ks[17], (DEPTH, D_MODEL, N_EXPERTS), D_MODEL ** -0.5),
        "b_router": nrm(ks[18], (DEPTH, N_EXPERTS), 0.01),
        "w_gate": nrm(ks[19], (DEPTH, N_EXPERTS, D_MODEL, EXPERT_FF), D_MODEL ** -0.5),
        "w_up": nrm(ks[20], (DEPTH, N_EXPERTS, D_MODEL, EXPERT_FF), D_MODEL ** -0.5),
        "w_down": nrm(ks[21], (DEPTH, N_EXPERTS, EXPERT_FF, D_MODEL), EXPERT_FF ** -0.5),
        "final_norm_g": 1.0 + nrm(ks[22], (D_MODEL,), 0.05),
    }


def reference(x, c, ctx, c_ctx, w_mod, b_mod, norm1_g, norm2_g, w_in, conv_w, a_log, dt_bias,
              gdn_norm_g, gm_norm_g, gm_ws, gm_bs, w_out, w_router, b_router, w_gate, w_up, w_down,
              final_norm_g):
    B, T, _ = x.shape
    rows = T // GRID_W
    n_lat_chunks = rows // ROWS_PER_CHUNK
    n_ctx_chunks = ctx.shape[1] // A_CHUNK
    h_lat, h_ctx = x, ctx
    for layer in range(DEPTH):
        last = layer == DEPTH - 1
        mod = jax.nn.silu(c) @ w_mod[layer] + b_mod[layer]
        mod_c = jax.nn.silu(c_ctx) @ w_mod[layer] + b_mod[layer]
        sh1, sc1, gt1, sh2, sc2, gt2 = jnp.split(mod[:, None, :], N_MOD, axis=-1)
        csh1, csc1, cgt1, csh2, csc2, cgt2 = jnp.split(mod_c, N_MOD, axis=-1)
        wl = w_in[layer]

        c_in = rms_norm(h_ctx, norm1_g[layer]) * (1.0 + csc1) + csh1
        p_ctx = c_in @ (wl[:, :N_STATE_COLS] if last else wl)
        qc, kc, vc, gc, bc = gdn_streams(p_ctx[..., :N_STATE_COLS], conv_w[layer], a_log[layer], dt_bias[layer])
        zero_state = jnp.zeros((B, B_HEADS, B_HEAD_DIM, B_HEAD_DIM), jnp.float32)
        o_ctx, s_f, s_b = gdn_two_dirs(qc, kc, vc, gc, bc, zero_state, zero_state)

        a_in = rms_norm(h_lat, norm1_g[layer]) * (1.0 + sc1) + sh1
        p_lat = a_in @ wl
        ql, kl, vl, gl, bl = gdn_streams(p_lat[..., :N_STATE_COLS], conv_w[layer], a_log[layer], dt_bias[layer])
        o_lat, _, _ = gdn_two_dirs(ql, kl, vl, gl, bl, s_f, s_b)
        h_lat = h_lat + gt1 * mix_out(p_lat, o_lat, n_lat_chunks, gdn_norm_g[layer], gm_norm_g[layer],
                                      gm_ws[layer], gm_bs[layer], w_out[layer])
        f_in = rms_norm(h_lat, norm2_g[layer]) * (1.0 + sc2) + sh2
        h_lat = h_lat + gt2 * expert_choice_ffn(f_in, w_router[layer], b_router[layer],
                                                w_gate[layer], w_up[layer], w_down[layer])

        if not last:
            h_ctx = h_ctx + cgt1 * mix_out(p_ctx, o_ctx, n_ctx_chunks, gdn_norm_g[layer], gm_norm_g[layer],
                                           gm_ws[layer], gm_bs[layer], w_out[layer])
            cf_in = rms_norm(h_ctx, norm2_g[layer]) * (1.0 + csc2) + csh2
            h_ctx = h_ctx + cgt2 * expert_choice_ffn(cf_in, w_router[layer], b_router[layer],
                                                     w_gate[layer], w_up[layer], w_down[layer])
    return rms_norm(h_lat, final_norm_g)
```

```python
from contextlib import ExitStack
import numpy as np
import concourse.bass as bass
import concourse.mybir as mybir
from concourse.bass_utils import run_bass_kernel_spmd

F32 = mybir.dt.float32
BF16 = mybir.dt.bfloat16
I32 = mybir.dt.int32
ALU = mybir.AluOpType
AF = mybir.ActivationFunctionType
AX = mybir.AxisListType

D = 1024
T = 8192
CTX = 256
NT = 66
NE = 16
CAP = 1024
SLOTS = 320
EPS = 1e-6


class Reg:
    __slots__ = ("w", "rs")

    def __init__(self):
        self.w = None
        self.rs = {}


class FW:
    NDMA = 48

    def __init__(self, nc, stack):
        self.nc = nc
        self.stack = stack
        self.engs = {"pe": nc.tensor, "dve": nc.vector, "act": nc.scalar,
                     "pool": nc.gpsimd, "sp": nc.sync}
        self.semh = {}
        self.cnt = {}
        self.known = {e: {} for e in self.engs}
        for e in self.engs:
            self.semh[e] = stack.enter_context(nc.semaphore("p_" + e))
            self.cnt[e] = 0
        self.dma_cnt = []
        for i in range(self.NDMA):
            self.semh[("d", i)] = stack.enter_context(nc.semaphore("d%d" % i))
            self.dma_cnt.append(0)
        self.dma_rr = 0
        self.scopes = []

    def sb(self, name, shape, dt=F32):
        stk = self.scopes[-1] if self.scopes else self.stack
        return stk.enter_context(self.nc.sbuf_tensor("s_" + name, list(shape), dt))

    def push_scope(self):
        self.scopes.append(ExitStack())

    def pop_scope(self):
        self.barrier()
        self.scopes.pop().close()

    def barrier(self):
        for e in self.engs:
            for e2 in self.engs:
                if e2 != e and self.cnt[e2] > 0:
                    self.wait(e, (e2, self.cnt[e2]))
            for i in range(self.NDMA):
                if self.dma_cnt[i] > 0:
                    self.wait(e, (("d", i), 16 * self.dma_cnt[i]))

    def ps(self, name, shape, dt=F32):
        return self.stack.enter_context(self.nc.psum_tensor("ps_" + name, list(shape), dt))

    def wait(self, eng, tok):
        key, val = tok
        if key == eng and eng == "pe":
            return
        if self.known[eng].get(key, 0) >= val:
            return
        self.engs[eng].wait_ge(self.semh[key], val)
        self.known[eng][key] = val

    def _deps(self, eng, reads, writes):
        deps = {}
        for r in reads:
            if r.w is not None:
                k, v = r.w
                if deps.get(k, 0) < v:
                    deps[k] = v
        for w in writes:
            if w.w is not None:
                k, v = w.w
                if deps.get(k, 0) < v:
                    deps[k] = v
            for k, v in w.rs.items():
                if deps.get(k, 0) < v:
                    deps[k] = v
        for k, v in deps.items():
            self.wait(eng, (k, v))

    def _commit(self, tok, reads, writes):
        k, v = tok
        for r in reads:
            if r.rs.get(k, 0) < v:
                r.rs[k] = v
        for w in writes:
            w.w = tok
            w.rs = {}

    def op(self, eng, fn, reads=(), writes=()):
        self._deps(eng, reads, writes)
        ins = fn(self.engs[eng])
        self.cnt[eng] += 1
        ins.then_inc(self.semh[eng], 1)
        tok = (eng, self.cnt[eng])
        self._commit(tok, reads, writes)
        return tok

    def dma(self, eng, fn, reads=(), writes=()):
        i = self.dma_rr
        self.dma_rr = (self.dma_rr + 1) % self.NDMA
        key = ("d", i)
        if self.dma_cnt[i] > 0:
            self.wait(eng, (key, 16 * self.dma_cnt[i]))
        self._deps(eng, reads, writes)
        ins = fn(self.engs[eng])
        self.dma_cnt[i] += 1
        ins.then_inc(self.semh[key], 16)
        tok = (key, 16 * self.dma_cnt[i])
        self._commit(tok, reads, writes)
        return tok

    def cc(self, fn, reads=(), writes=()):
        if "cc" not in self.semh:
            self.semh["cc"] = self.stack.enter_context(self.nc.semaphore("cc_sem"))
            self.cnt["cc"] = 0
        if self.cnt["cc"] > 0:
            self.wait("pool", ("cc", self.cnt["cc"]))
        self._deps("pool", reads, writes)
        ins = fn(self.engs["pool"])
        self.cnt["cc"] += 1
        ins.then_inc(self.semh["cc"])
        tok = ("cc", self.cnt["cc"])
        self._commit(tok, reads, writes)
        return tok

    def finish(self, regs=()):
        for r in regs:
            if r.w is not None:
                self.wait("sp", r.w)
        for i in range(self.NDMA):
            if self.dma_cnt[i] > 0:
                self.wait("sp", (("d", i), 16 * self.dma_cnt[i]))
        for e2 in self.engs:
            if e2 != "sp" and self.cnt[e2] > 0:
                self.wait("sp", (e2, self.cnt[e2]))
        if self.cnt.get("cc", 0) > 0:
            self.wait("sp", ("cc", self.cnt["cc"]))


def build_program(stage=99, dbg=False):
    nc = bass.Bass("TRN2", target_bir_lowering=False)

    def din(name, shape, dt=F32):
        return nc.dram_tensor(name, list(shape), dt, kind="ExternalInput").ap()

    x_d = din("x", [T, D])
    ctx_d = din("ctx", [CTX, D])
    crow_d = din("crow", [1, 2 * D])
    wmod_d = din("w_mod", [D, 6 * D])
    bmod_d = din("b_mod", [1, 6 * D])
    nrm_d = din("nrm", [1, 3 * D])
    win_d = din("w_in", [D, 772])
    small_d = din("small", [1, 1152 + 4 + 128 + 128 + 128])
    gmws_d = din("gm_ws", [128, 128])
    wout_d = din("w_out", [D, D])
    cst_d = din("cst", [128, 6 * 128])
    xown_d = din("x_own", [2048, D])
    yidx_d = din("yidx", [128, 8], I32)
    wr_d = din("w_router", [D, NE])
    br_d = din("b_router", [1, NE])
    wg_d = din("w_gate", [NE, D, D])
    wu_d = din("w_up", [NE, D, D])
    wd_d = din("w_down", [NE, D, D])
    rc_d = din("rcst", [128, 16 + SLOTS])
    out_d = nc.dram_tensor("out", [2048, D], F32, kind="ExternalOutput").ap()
    dbg_d = {}

    def dout(name, shape, dt=F32):
        dbg_d[name] = nc.dram_tensor(name, list(shape), dt, kind="ExternalOutput").ap()
        return dbg_d[name]

    PRE_W = 8452
    pre_d = nc.dram_tensor("pre_scr", [3, 128, PRE_W], F32).ap()
    modrow_d = nc.dram_tensor("modrow_scr", [128, 6 * D], F32).ap()

    with ExitStack() as st:
        fw = FW(nc, st)
        sb, ps = fw.sb, fw.ps
        P = lambda fn, r=(), w=(): fw.op("pe", fn, r, w)
        V = lambda fn, r=(), w=(): fw.op("dve", fn, r, w)
        A = lambda fn, r=(), w=(): fw.op("act", fn, r, w)
        G = lambda fn, r=(), w=(): fw.op("pool", fn, r, w)

        def run_rr(gens):
            gens = list(gens)
            while gens:
                for g_ in list(gens):
                    try:
                        next(g_)
                    except StopIteration:
                        gens.remove(g_)

        cst = sb("cst", [128, 6 * 128]); Rc = Reg()
        fw.dma("sp", lambda e: e.dma_start(out=cst[:], in_=cst_d), writes=[Rc])
        ident = cst[:, 0:128]
        U_in = cst[:, 128:256]
        L_in = cst[:, 256:384]
        U_st = cst[:, 384:512]
        L_st = cst[:, 512:640]
        ones = cst[:, 640:768]
        identb = sb("identb", [128, 128], BF16)
        V(lambda e: e.tensor_copy(out=identb[:], in_=ident), [Rc], [Rc])
        epst = sb("epst", [128, 1])
        V(lambda e: e.memset(epst[:], EPS), [], [Rc])
        onec = sb("onec", [128, 1])
        V(lambda e: e.memset(onec[:], 1.0), [], [Rc])

        bank = [ps("bank%d" % i, [128, 512]) for i in range(8)]
        Rb = [Reg() for _ in range(8)]

        fm = sb("fm", [128, 16, 2]); Rfm = Reg()
        g1 = sb("g1", [128, 8]); Rg1 = Reg()
        s1 = sb("s1", [128, 8, 2]); Rs1 = Reg()
        cw = sb("cw", [128, 16]); Rcw = Reg()
        srow = sb("srow", [128, 388]); Rsrow = Reg()
        nexpA = sb("nexpA", [128, 2]); RnA = Reg()
        fw.push_scope()
        uT = sb("uT", [128, T], BF16); RuT = Reg()
        zs = sb("zs", [128, 64, 128], BF16); Rzs = Reg()
        vgn = sb("vgn", [128, 64, 128], BF16); Rvgn = Reg()
        ab = sb("ab", [128, NT, 4]); Rab = Reg()
        fw.push_scope()
        wsb = sb("wsb", [128, 8, 772], BF16); Rw = Reg()
        fw.push_scope()
        rows = sb("rows", [128, 4 * D]); Rrows = Reg()
        g2F = sb("g2F", [128, 2 * D]); Rg2F = Reg()
        crow = sb("crow", [1, 2 * D]); Rcrow = Reg()
        fw.dma("sp", lambda e: e.dma_start(out=crow[:], in_=crow_d), writes=[Rcrow])
        bmod = sb("bmod", [1, 6 * D]); Rbm = Reg()
        fw.dma("sp", lambda e: e.dma_start(out=bmod[:], in_=bmod_d), writes=[Rbm])
        nrm = sb("nrm", [1, 3 * D]); Rnrm = Reg()
        fw.dma("sp", lambda e: e.dma_start(out=nrm[:], in_=nrm_d), writes=[Rnrm])
        small = sb("small", [1, 1540]); Rsm = Reg()
        fw.dma("sp", lambda e: e.dma_start(out=small[:], in_=small_d), writes=[Rsm])
        fw.dma("pool", lambda e: e.dma_start(out=wsb[:], in_=win_d.rearrange("(j k) n -> k j n", k=128)), writes=[Rw])

        A(lambda e: e.activation(out=crow[:], in_=crow[:], func=AF.Silu), [Rcrow], [Rcrow])
        scv = sb("scv", [128, 8, 2]); Rscv = Reg()
        for j in range(8):
            for n in range(2):
                P(lambda e: e.matmul(bank[0][:, j * 2 + n:j * 2 + n + 1], lhsT=crow[0:1, n * D + j * 128:n * D + (j + 1) * 128],
                                     rhs=ones[0:1, 0:1], start=True, stop=True), [Rcrow, Rc], [Rb[0]])
        V(lambda e: e.tensor_copy(out=scv[:].rearrange("k j n -> k (j n)"), in_=bank[0][:, 0:16]), [Rb[0]], [Rscv])
        scb = sb("scb", [128, 8, 128]); Rscb = Reg()
        for j in range(8):
            V(lambda e: e.tensor_copy(out=scb[:, j, :], in_=scv[:, j, 0:1].to_broadcast([128, 128])), [Rscv], [Rscb])

        wm = [sb("wm%d" % i, [128, 8, 512]) for i in range(2)]
        Rwm = [Reg(), Reg()]
        wmod_v = wmod_d.rearrange("(j k) n -> k j n", k=128)
        for pc in range(12):
            s = pc % 2
            fw.dma("sp", lambda e: e.dma_start(out=wm[s][:], in_=wmod_v[:, :, pc * 512:(pc + 1) * 512]), writes=[Rwm[s]])
            bk = 1 + (pc % 2)
            if pc < 4:
                for m in range(4):
                    for j in range(8):
                        P(lambda e: e.matmul(bank[bk][:, m * 2:m * 2 + 2], lhsT=wm[s][:, j, m * 128:(m + 1) * 128], rhs=scv[:, j, :],
                                             start=(j == 0), stop=False), [Rwm[s], Rscv], [Rb[bk]])
                    c0 = pc * 512 + m * 128
                    P(lambda e: e.matmul(bank[bk][:, m * 2:m * 2 + 2], lhsT=bmod[0:1, c0:c0 + 128], rhs=ones[0:1, 0:2],
                                         start=False, stop=True), [Rbm, Rc], [Rb[bk]])
                V(lambda e: e.tensor_copy(out=fm[:, pc * 4:(pc + 1) * 4, :].rearrange("k a n -> k (a n)"), in_=bank[bk][:, 0:8]),
                  [Rb[bk]], [Rfm])
            else:
                for j in range(8):
                    P(lambda e: e.matmul(bank[bk][:, :], lhsT=scb[:, j, :], rhs=wm[s][:, j, :], start=(j == 0), stop=False),
                      [Rwm[s], Rscb], [Rb[bk]])
                c0 = pc * 512
                P(lambda e: e.matmul(bank[bk][:, :], lhsT=ones[0:1, :], rhs=bmod[0:1, c0:c0 + 512], start=False, stop=True),
                  [Rbm, Rc], [Rb[bk]])
                A(lambda e: e.activation(out=rows[:, (pc - 4) * 512:(pc - 3) * 512], in_=bank[bk][:, :], func=AF.Copy), [Rb[bk]], [Rrows])
        for j in range(8):
            P(lambda e: e.matmul(bank[3][:, j:j + 1], lhsT=nrm[0:1, j * 128:(j + 1) * 128], rhs=ones[0:1, 0:1], start=True, stop=True),
              [Rnrm, Rc], [Rb[3]])
        V(lambda e: e.tensor_copy(out=g1[:], in_=bank[3][:, 0:8]), [Rb[3]], [Rg1])
        for n in range(2):
            V(lambda e: e.scalar_tensor_tensor(out=s1[:, :, n], in0=fm[:, 8:16, n], scalar=1.0, in1=g1[:], op0=ALU.add, op1=ALU.mult),
              [Rfm, Rg1], [Rs1])
        for q in range(4):
            P(lambda e: e.matmul(bank[4][:, :], lhsT=ones[0:1, :], rhs=nrm[0:1, D + q * 512:D + (q + 1) * 512], start=True, stop=True),
              [Rnrm, Rc], [Rb[4]])
            A(lambda e: e.activation(out=g2F[:, q * 512:(q + 1) * 512], in_=bank[4][:, :], func=AF.Copy), [Rb[4]], [Rg2F])
        V(lambda e: e.scalar_tensor_tensor(out=rows[:, 2 * D:3 * D], in0=rows[:, 2 * D:3 * D], scalar=1.0, in1=g2F[:, 0:D],
                                           op0=ALU.add, op1=ALU.mult), [Rrows, Rg2F], [Rrows])
        for sidx in range(3):
            for tap in range(3):
                c0 = tap * 384 + sidx * 128
                P(lambda e: e.matmul(bank[5][:, sidx * 3 + tap:sidx * 3 + tap + 1], lhsT=small[0:1, c0:c0 + 128], rhs=ones[0:1, 0:1],
                                     start=True, stop=True), [Rsm, Rc], [Rb[5]])
        P(lambda e: e.matmul(bank[5][:, 9:10], lhsT=small[0:1, 1156:1284], rhs=ones[0:1, 0:1], start=True, stop=True), [Rsm, Rc], [Rb[5]])
        V(lambda e: e.tensor_copy(out=cw[:, 0:10], in_=bank[5][:, 0:10]), [Rb[5]], [Rcw])
        P(lambda e: e.matmul(bank[6][:, 0:388], lhsT=ones[0:1, :], rhs=small[0:1, 1152:1540], start=True, stop=True), [Rsm, Rc], [Rb[6]])
        V(lambda e: e.tensor_copy(out=srow[:], in_=bank[6][:, 0:388]), [Rb[6]], [Rsrow])
        A(lambda e: e.activation(out=nexpA[:], in_=srow[:, 0:2], func=AF.Exp), [Rsrow], [RnA])
        V(lambda e: e.tensor_scalar(out=nexpA[:], in0=nexpA[:], scalar1=-1.0, scalar2=None, op0=ALU.mult), [RnA], [RnA])
        Rmodrow = Reg()
        fw.dma("sp", lambda e: e.dma_start(out=modrow_d[:, 0:4 * D], in_=rows[:]), [Rrows], [Rmodrow])
        fw.dma("sp", lambda e: e.dma_start(out=modrow_d[:, 4 * D:6 * D], in_=g2F[:]), [Rg2F], [Rmodrow])

        if dbg:
            d = dout("dbg_fm", [128, 32]); Rd = Reg()
            fw.dma("sp", lambda e: e.dma_start(out=d, in_=fm[:].rearrange("k a n -> k (a n)")), [Rfm], [Rd])
            d2 = dout("dbg_rows", [128, 4 * D])
            fw.dma("sp", lambda e: e.dma_start(out=d2, in_=rows[:]), [Rrows], [Rd])
            d3 = dout("dbg_s1", [128, 16])
            fw.dma("sp", lambda e: e.dma_start(out=d3, in_=s1[:].rearrange("k a n -> k (a n)")), [Rs1], [Rd])

        fw.pop_scope()
        fw.push_scope()
        xt = [sb("xt%d" % i, [128, D]) for i in range(2)]; Rxt = [Reg(), Reg()]
        junk = sb("junk", [128, D]); Rjunk = Reg()
        stat = [sb("stat%d" % i, [128, 4]) for i in range(2)]; Rstat = [Reg(), Reg()]
        xn = [sb("xn%d" % i, [128, D], BF16) for i in range(2)]; Rxn = [Reg(), Reg()]
        aT = [sb("aT%d" % i, [128, 8, 512], BF16) for i in range(2)]; RaTt = [[Reg() for _ in range(4)] for _ in range(2)]
        prew = [sb("prew%d" % i, [128, 3, 512]) for i in range(2)]; Rprew = [Reg(), Reg()]
        tmg4 = [sb("tmg4_%d" % i, [128, 256]) for i in range(4)]; Rtmg4 = [Reg() for _ in range(4)]
        st4 = sb("st4", [128, 4, 4]); Rst4 = [Reg() for _ in range(4)]
        zero = sb("zero", [128, 4]); Rz = Reg()
        V(lambda e: e.memset(zero[:], 0.0), [], [Rz])
        Rpre = Reg()
        for sidx in range(3):
            for col in (0, 257, 258, 8451):
                fw.dma("sp", lambda e: e.dma_start(out=pre_d[sidx, :, col:col + 1], in_=zero[:, 0:1], allow_slow_non_contiguous=True), [Rz], [Rpre])

        blocks = [(ctx_d, 0, 2, 1, 0, 1)]
        for bi in range(16):
            blocks.append((x_d, bi * 512, 4, 0, 2 + bi * 4, 259 + bi * 512))
        tcount = 0
        for bidx, (src, row0, nb, mn, t0, pc0) in enumerate(blocks):
            bs = bidx % 2
            def a1_tile(ti, s, bkT):
                r0 = row0 + ti * 128
                fw.dma("sp", lambda e: e.dma_start(out=xt[s][:], in_=src[r0:r0 + 128, :]), writes=[Rxt[s]])
                yield
                A(lambda e: e.activation(out=junk[:], in_=xt[s][:], func=AF.Square, accum_out=stat[s][:, 0:1]), [Rxt[s]], [Rjunk, Rstat[s]])
                yield
                A(lambda e: e.activation(out=stat[s][:, 1:2], in_=stat[s][:, 0:1], func=AF.Sqrt, scale=1.0 / D, bias=epst[:, 0:1]),
                  [Rstat[s], Rc], [Rstat[s]])
                yield
                V(lambda e: e.reciprocal(out=stat[s][:, 2:3], in_=stat[s][:, 1:2]), [Rstat[s]], [Rstat[s]])
                yield
                A(lambda e: e.activation(out=xn[s][:], in_=xt[s][:], func=AF.Copy, scale=stat[s][:, 2:3]), [Rxt[s], Rstat[s]], [Rxn[s]])
                yield
                pT = bank[bkT][:, :].bitcast(BF16)
                for j in range(8):
                    P(lambda e: e.transpose(out=pT[:, j * 128:(j + 1) * 128], in_=xn[s][:, j * 128:(j + 1) * 128], identity=identb[:]),
                      [Rxn[s], Rc], [Rb[bkT]])
                yield
                for j in range(8):
                    if j % 4 != 3:
                        V(lambda e: e.tensor_scalar(out=aT[bs][:, j, ti * 128:(ti + 1) * 128], in0=pT[:, j * 128:(j + 1) * 128],
                                                    scalar1=s1[:, j, mn:mn + 1], scalar2=fm[:, j, mn:mn + 1], op0=ALU.mult, op1=ALU.add),
                          [Rb[bkT], Rs1, Rfm], [RaTt[bs][ti]])
                    else:
                        A(lambda e: e.activation(out=aT[bs][:, j, ti * 128:(ti + 1) * 128], in_=pT[:, j * 128:(j + 1) * 128],
                                                 func=AF.Identity, scale=s1[:, j, mn:mn + 1], bias=fm[:, j, mn:mn + 1]),
                          [Rb[bkT], Rs1, Rfm], [RaTt[bs][ti]])
            for t2 in range(0, nb, 2):
                run_rr([a1_tile(t2, 0, 7), a1_tile(t2 + 1, 1, 6)])
            N = nb * 128
            for gi in range(4):
                bk = gi % 2
                for j in range(8):
                    P(lambda e: e.matmul(bank[bk][:, 0:N], lhsT=wsb[:, j, gi * 128:(gi + 1) * 128], rhs=aT[bs][:, j, 0:N],
                                         start=(j == 0), stop=(j == 7)), [Rw] + RaTt[bs][0:nb], [Rb[bk]])
                if gi < 3:
                    if gi % 2 == 0:
                        V(lambda e: e.tensor_copy(out=prew[bs][:, gi, 0:N], in_=bank[bk][:, 0:N]), [Rb[bk]], [Rprew[bs]])
                    else:
                        A(lambda e: e.activation(out=prew[bs][:, gi, 0:N], in_=bank[bk][:, 0:N], func=AF.Copy), [Rb[bk]], [Rprew[bs]])
                elif mn == 0:
                    A(lambda e: e.activation(out=uT[:, row0:row0 + N], in_=bank[bk][:, 0:N], func=AF.Gelu), [Rb[bk]], [RuT])
            fw.dma("sp", lambda e: e.dma_start(out=pre_d[:, :, pc0:pc0 + N].rearrange("s p n -> p s n"), in_=prew[bs][:, :, 0:N]),
                   [Rprew[bs]], [Rpre])
            def tm_tile(ti):
                tix = t0 + ti
                bk = 2 + ti
                for j in range(8):
                    P(lambda e: e.matmul(bank[bk][:, 0:260], lhsT=aT[bs][:, j, ti * 128:(ti + 1) * 128], rhs=wsb[:, j, 512:772],
                                         start=(j == 0), stop=(j == 7)), [Rw] + RaTt[bs][0:nb], [Rb[bk]])
                yield
                V(lambda e: e.tensor_copy(out=ab[:, tix, :], in_=bank[bk][:, 256:260]), [], [Rab, Rb[bk]])
                if mn != 0:
                    return
                lt = tix - 2
                yield
                A(lambda e: e.activation(out=zs[:, lt, :], in_=bank[bk][:, 0:128], func=AF.Silu), [], [Rzs, Rb[bk]])
                yield
                A(lambda e: e.activation(out=tmg4[ti][:, 0:128], in_=bank[bk][:, 128:256], func=AF.Gelu), [], [Rtmg4[ti], Rb[bk]])
                yield
                V(lambda e: e.tensor_tensor(out=tmg4[ti][:, 128:256], in0=tmg4[ti][:, 0:128], in1=tmg4[ti][:, 0:128], op=ALU.mult), [Rtmg4[ti]], [Rtmg4[ti]])
                yield
                V(lambda e: e.tensor_reduce(out=st4[:, ti, 0:1], in_=tmg4[ti][:, 128:256], axis=AX.X, op=ALU.add), [Rtmg4[ti]], [Rst4[ti]])
                yield
                A(lambda e: e.activation(out=st4[:, ti, 1:2], in_=st4[:, ti, 0:1], func=AF.Sqrt, scale=1.0 / 128, bias=epst[:, 0:1]),
                  [Rst4[ti], Rc], [Rst4[ti]])
                yield
                V(lambda e: e.reciprocal(out=st4[:, ti, 2:3], in_=st4[:, ti, 1:2]), [Rst4[ti]], [Rst4[ti]])
                yield
                V(lambda e: e.scalar_tensor_tensor(out=vgn[:, lt, :], in0=tmg4[ti][:, 0:128], scalar=st4[:, ti, 2:3], in1=srow[:, 132:260],
                                                   op0=ALU.mult, op1=ALU.mult), [Rtmg4[ti], Rst4[ti], Rsrow], [Rvgn])
            run_rr([tm_tile(ti) for ti in range(nb)])
        if dbg:
            Rd = Reg()
            d = dout("dbg_ab", [128, NT * 4])
            fw.dma("sp", lambda e: e.dma_start(out=d, in_=ab[:].rearrange("p t c -> p (t c)")), [Rab], [Rd])
            d = dout("dbg_uT", [128, T], BF16)
            fw.dma("sp", lambda e: e.dma_start(out=d, in_=uT[:]), [RuT], [Rd])
            d = dout("dbg_zs", [128, 64 * 128], BF16)
            fw.dma("sp", lambda e: e.dma_start(out=d, in_=zs[:].rearrange("p t c -> p (t c)")), [Rzs], [Rd])
            d = dout("dbg_vgn", [128, 64 * 128], BF16)
            fw.dma("sp", lambda e: e.dma_start(out=d, in_=vgn[:].rearrange("p t c -> p (t c)")), [Rvgn], [Rd])
            d = dout("dbg_pre", [3, 128, PRE_W])
            fw.dma("sp", lambda e: e.dma_start(out=d, in_=pre_d), [Rpre], [Rd])
            fw.finish([Rd])
        fw.pop_scope()
        fw.pop_scope()
        if stage >= 2:
            oacc = sb("oacc", [128, 64, 128]); Roacc = [Reg() for _ in range(64)]
            fw.push_scope()
            qT = sb("qT", [128, NT * 128], BF16); RqT = Reg()
            kT = sb("kT", [128, NT * 128], BF16); RkT = Reg()
            ktok = sb("ktok", [128, NT, 128], BF16); Rktok = Reg()
            vtok = sb("vtok", [128, NT, 128], BF16); Rvtok = Reg()
            fw.push_scope()
            pre3 = [sb("pre3_%d" % i, [128, 3, 514]) for i in range(2)]; Rpre3 = [Reg(), Reg()]
            cacc = [[sb("cacc%d_%d" % (p_, i), [128, 512]) for i in range(3)] for p_ in range(2)]; Rcacc = [[Reg() for _ in range(3)] for _ in range(2)]
            sl = [[sb("sl%d_%d" % (p_, i), [128, 512]) for i in range(2)] for p_ in range(2)]; Rsl = [[Reg() for _ in range(2)] for _ in range(2)]
            sq = [[sb("sq%d_%d" % (p_, i), [128, 512]) for i in range(2)] for p_ in range(2)]; Rsq = [[Reg() for _ in range(2)] for _ in range(2)]
            rn = [[sb("rn%d_%d" % (p_, i), [128, 512]) for i in range(2)] for p_ in range(2)]; Rrn = [[Reg() for _ in range(2)] for _ in range(2)]
            vbf = [sb("vbf%d" % p_, [128, 512], BF16) for p_ in range(2)]; Rvbf = [Reg(), Reg()]
            blocks2 = [(1, 256, 0)] + [(259 + bi * 512, 512, 2 + bi * 4) for bi in range(16)]

            def a2_stream(bidx, sidx):
                c0, N, t0 = blocks2[bidx]
                p_ = bidx % 2
                ca, Rca = cacc[p_][sidx], Rcacc[p_][sidx]
                if sidx == 0:
                    fw.dma("sp", lambda e: e.dma_start(out=pre3[p_][:, :, 0:N + 2], in_=pre_d[:, :, c0 - 1:c0 + N + 1].rearrange("s p n -> p s n")),
                           [Rpre], [Rpre3[p_]])
                yield
                V(lambda e: e.tensor_scalar(out=ca[:, 0:N], in0=pre3[p_][:, sidx, 0:N], scalar1=cw[:, sidx * 3:sidx * 3 + 1],
                                            scalar2=None, op0=ALU.mult), [Rpre3[p_], Rcw], [Rca])
                for tap in (1, 2):
                    yield
                    V(lambda e: e.scalar_tensor_tensor(out=ca[:, 0:N], in0=pre3[p_][:, sidx, tap:tap + N],
                                                       scalar=cw[:, sidx * 3 + tap:sidx * 3 + tap + 1], in1=ca[:, 0:N],
                                                       op0=ALU.mult, op1=ALU.add), [Rpre3[p_], Rcw, Rca], [Rca])
                yield
                if sidx == 2:
                    bkv = 7 if p_ == 0 else 3
                    A(lambda e: e.activation(out=vbf[p_][:, 0:N], in_=ca[:, 0:N], func=AF.Silu), [Rca], [Rvbf[p_]])
                    yield
                    pTb = bank[bkv][:, :].bitcast(BF16)
                    for ti in range(N // 128):
                        P(lambda e: e.transpose(out=pTb[:, ti * 128:(ti + 1) * 128], in_=vbf[p_][:, ti * 128:(ti + 1) * 128], identity=identb[:]), [Rvbf[p_], Rc], [Rb[bkv]])
                    yield
                    V(lambda e: e.tensor_copy(out=vtok[:, t0:t0 + N // 128, :], in_=pTb[:, 0:N].rearrange("p (t c) -> p t c", c=128)), [Rb[bkv]], [Rvtok])
                    return
                bk = (4 + sidx) if p_ == 0 else sidx
                A(lambda e: e.activation(out=sl[p_][sidx][:, 0:N], in_=ca[:, 0:N], func=AF.Silu), [Rca], [Rsl[p_][sidx]])
                yield
                A(lambda e: e.activation(out=sq[p_][sidx][:, 0:N], in_=sl[p_][sidx][:, 0:N], func=AF.Square), [Rsl[p_][sidx]], [Rsq[p_][sidx]])
                yield
                P(lambda e: e.matmul(bank[bk][:, 0:N], lhsT=ones, rhs=sq[p_][sidx][:, 0:N], start=True, stop=True), [Rsq[p_][sidx], Rc], [Rb[bk]])
                yield
                A(lambda e: e.activation(out=rn[p_][sidx][:, 0:N], in_=bank[bk][:, 0:N], func=AF.Sqrt, bias=epst[:, 0:1]), [Rb[bk], Rc], [Rrn[p_][sidx]])
                yield
                V(lambda e: e.reciprocal(out=rn[p_][sidx][:, 0:N], in_=rn[p_][sidx][:, 0:N]), [Rrn[p_][sidx]], [Rrn[p_][sidx]])
                yield
                dst, Rdst = (qT, RqT) if sidx == 0 else (kT, RkT)
                scl = 128.0 ** -0.5 if sidx == 0 else 1.0
                V(lambda e: e.scalar_tensor_tensor(out=dst[:, t0 * 128:t0 * 128 + N], in0=sl[p_][sidx][:, 0:N], scalar=scl, in1=rn[p_][sidx][:, 0:N],
                                                   op0=ALU.mult, op1=ALU.mult), [Rsl[p_][sidx], Rrn[p_][sidx]], [Rdst])
                if sidx == 1:
                    yield
                    bkk = 6 if p_ == 0 else 2
                    pTb = bank[bkk][:, :].bitcast(BF16)
                    for ti in range(N // 128):
                        P(lambda e: e.transpose(out=pTb[:, ti * 128:(ti + 1) * 128], in_=kT[:, (t0 + ti) * 128:(t0 + ti + 1) * 128], identity=identb[:]),
                          [RkT, Rc], [Rb[bkk]])
                    yield
                    A(lambda e: e.activation(out=ktok[:, t0:t0 + N // 128, :], in_=pTb[:, 0:N].rearrange("p (t c) -> p t c", c=128), func=AF.Copy), [Rb[bkk]], [Rktok])

            run_rr([a2_stream(0, 0), a2_stream(0, 1), a2_stream(0, 2)])
            for b2 in range(1, 17, 2):
                run_rr([a2_stream(bb, ss) for bb in (b2, b2 + 1) for ss in range(3)])
            fw.pop_scope()
            import os as _os
            if _os.environ.get("KSTOP") == "a2":
                Ro = Reg()
                fw.dma("sp", lambda e: e.dma_start(out=out_d[0:128, 0:32], in_=fm[:].rearrange("k a n -> k (a n)")), [Rfm, RqT, RkT, Rktok, Rvtok], [Ro])
                fw.finish([Ro])
                return nc, list(dbg_d.keys())
            gsc = {}
            Rgs = Reg()
            abT = sb("abT", [128, 4, NT]); RabT = Reg()
            for c_ in range(4):
                V(lambda e: e.tensor_copy(out=abT[:, c_, :], in_=ab[:, :, c_]), [Rab], [RabT])
            for d in range(2):
                for nm in ("g", "beta", "gc", "neggc", "eg", "ekg", "egl", "bkg", "negbeta", "tmp"):
                    gsc[(nm, d)] = sb("gs_%s%d" % (nm, d), [128, NT])
                gs = lambda nm: gsc[(nm, d)]
                A(lambda e: e.activation(out=gs("tmp")[:], in_=abT[:, d, :], func=AF.Exp, bias=srow[:, 2 + d:3 + d]), [RabT, Rsrow], [Rgs])
                A(lambda e: e.activation(out=gs("tmp")[:], in_=gs("tmp")[:], func=AF.Ln, bias=onec[:, 0:1]), [Rgs, Rc], [Rgs])
                V(lambda e: e.tensor_scalar(out=gs("g")[:], in0=gs("tmp")[:], scalar1=nexpA[:, d:d + 1], scalar2=None, op0=ALU.mult), [Rgs, RnA], [Rgs])
                A(lambda e: e.activation(out=gs("beta")[:], in_=abT[:, 2 + d, :], func=AF.Sigmoid), [RabT], [Rgs])
                tri = U_in if d == 0 else L_in
                P(lambda e: e.matmul(bank[0][:, 0:NT], lhsT=tri, rhs=gs("g")[:], start=True, stop=True), [Rgs, Rc], [Rb[0]])
                P(lambda e: e.matmul(bank[0][:, 128:128 + NT], lhsT=ones, rhs=gs("g")[:], start=True, stop=True), [Rgs, Rc], [Rb[0]])
                V(lambda e: e.tensor_copy(out=gs("gc")[:], in_=bank[0][:, 0:NT]), [Rb[0]], [Rgs])
                V(lambda e: e.tensor_scalar(out=gs("neggc")[:], in0=bank[0][:, 0:NT], scalar1=-1.0, scalar2=None, op0=ALU.mult), [Rb[0]], [Rgs])
                A(lambda e: e.activation(out=gs("eg")[:], in_=bank[0][:, 0:NT], func=AF.Exp), [Rb[0]], [Rgs])
                A(lambda e: e.activation(out=gs("egl")[:], in_=bank[0][:, 128:128 + NT], func=AF.Exp), [Rb[0]], [Rgs])
                V(lambda e: e.tensor_tensor(out=gs("tmp")[:], in0=bank[0][:, 128:128 + NT], in1=gs("gc")[:], op=ALU.subtract), [Rb[0], Rgs], [Rgs])
                A(lambda e: e.activation(out=gs("ekg")[:], in_=gs("tmp")[:], func=AF.Exp), [Rgs], [Rgs])
                V(lambda e: e.tensor_tensor(out=gs("bkg")[:], in0=gs("beta")[:], in1=gs("eg")[:], op=ALU.mult), [Rgs], [Rgs])
                V(lambda e: e.tensor_scalar(out=gs("negbeta")[:], in0=gs("beta")[:], scalar1=-1.0, scalar2=None, op0=ALU.mult), [Rgs], [Rgs])
            if dbg:
                Rd = Reg()
                d_ = dout("dbg_qT", [128, NT * 128], BF16)
                fw.dma("sp", lambda e: e.dma_start(out=d_, in_=qT[:]), [RqT], [Rd])
                d_ = dout("dbg_kT", [128, NT * 128], BF16)
                fw.dma("sp", lambda e: e.dma_start(out=d_, in_=kT[:]), [RkT], [Rd])
                d_ = dout("dbg_ktok", [128, NT * 128], BF16)
                fw.dma("sp", lambda e: e.dma_start(out=d_, in_=ktok[:].rearrange("p t c -> p (t c)")), [Rktok], [Rd])
                d_ = dout("dbg_vtok", [128, NT * 128], BF16)
                fw.dma("sp", lambda e: e.dma_start(out=d_, in_=vtok[:].rearrange("p t c -> p (t c)")), [Rvtok], [Rd])
                for nm in ("g", "beta", "gc", "egl", "ekg"):
                    for d in range(2):
                        d_ = dout("dbg_%s%d" % (nm, d), [128, NT])
                        fw.dma("sp", lambda e: e.dma_start(out=d_, in_=gsc[(nm, d)][:]), [Rgs], [Rd])
                fw.finish([Rd])
            fw.barrier()
            DTI = F32
            fw.push_scope()
            Rq = [[rb_] * 4 for rb_ in [Reg() for _ in range(8)]]
            qv = lambda b_, q_: bank[b_][:, q_ * 128:(q_ + 1) * 128]
            W = {}
            RW = {}
            PREPB = (("dgc", F32), ("dng", F32), ("m1", F32), ("m2", F32), ("E1m", F32), ("E2m", F32), ("N", DTI), ("Nt", DTI), ("N2", DTI),
                     ("Nt2", DTI), ("Qa", DTI), ("Qb", DTI), ("TT", BF16), ("kbg", BF16), ("vb", BF16))
            HOB = (("u", F32), ("wT", BF16), ("attnT", BF16), ("kg", BF16))
            SEQB = (("vnew", BF16), ("S", F32), ("Sb", BF16), ("otmp", F32), ("otmp2", F32))
            NCTX, NSLOT = 2, 3
            ALIAS = {}
            for d in range(2):
                for c_ in range(NCTX):
                    for nm, dt_ in PREPB:
                        if nm in ALIAS:
                            continue
                        W[(nm, d, c_)] = sb("w_%s%d_%d" % (nm, d, c_), [128, 128], dt_); RW[(nm, d, c_)] = Reg()
                    for nm, tgt in ALIAS.items():
                        W[(nm, d, c_)] = W[(tgt, d, c_)]; RW[(nm, d, c_)] = RW[(tgt, d, c_)]
                for sl_ in range(NSLOT):
                    for nm, dt_ in HOB:
                        W[(nm, d, "h", sl_)] = sb("h_%s%d_%d" % (nm, d, sl_), [128, 128], dt_); RW[(nm, d, "h", sl_)] = Reg()
                for nm, dt_ in SEQB:
                    W[(nm, d)] = sb("q_%s%d" % (nm, d), [128, 128], dt_); RW[(nm, d)] = Reg()
                V(lambda e: e.memset(W[("S", d)][:], 0.0), [], [RW[("S", d)]])
                V(lambda e: e.memset(W[("Sb", d)][:], 0.0), [], [RW[("Sb", d)]])
            identI = ident if DTI == F32 else identb[:]
            touched = set()
            HO = ("u", "wT", "attnT", "kg")
            bankregs = set(id(x[0]) for x in Rq)

            def excl(E):
                def f(fn, r=(), w=()):
                    return E(fn, [x for x in r if id(x) not in bankregs], list(w) + [x for x in r if id(x) in bankregs])
                return f
            V0 = V
            V, A = excl(V), excl(A)
            Rmk = Reg()
            nmT = [sb("nmT%d" % d_, [128, 128]) for d_ in range(2)]
            pmS = [sb("pmS%d" % d_, [128, 128]) for d_ in range(2)]
            for d_ in range(2):
                V0(lambda e: e.tensor_scalar(out=nmT[d_][:], in0=(U_in if d_ == 0 else L_in), scalar1=-1.0, scalar2=200.0, op0=ALU.add, op1=ALU.mult), [Rc], [Rmk])
                V0(lambda e: e.tensor_scalar(out=pmS[d_][:], in0=(L_st if d_ == 0 else U_st), scalar1=-1.0, scalar2=-200.0, op0=ALU.add, op1=ALU.mult), [Rc], [Rmk])

            def prep(n, d, c_, sl_):
                w = lambda nm: (W[(nm, d, "h", sl_)] if nm in HO else W[(nm, d, c_)])[:]
                r = lambda nm: RW[(nm, d, "h", sl_)] if nm in HO else RW[(nm, d, c_)]
                gs = lambda nm: gsc[(nm, d)][:, n:n + 1]
                bk = d * NCTX + c_
                Rk = Rq[bk][0]
                tk = slice(n * 128, (n + 1) * 128)
                P(lambda e: e.matmul(qv(bk, 0), lhsT=kT[:, tk], rhs=kT[:, tk], start=True, stop=True), [RkT], [Rk])
                P(lambda e: e.matmul(qv(bk, 1), lhsT=kT[:, tk], rhs=qT[:, tk], start=True, stop=True), [RkT, RqT], [Rk])
                V(lambda e: e.tensor_scalar(out=w("dgc"), in0=ident, scalar1=gs("gc"), scalar2=None, op0=ALU.mult), [Rc, Rgs], [r("dgc")])
                A(lambda e: e.activation(out=w("dng"), in_=ident, func=AF.Copy, scale=gs("neggc")), [Rc, Rgs], [r("dng")])
                A(lambda e: e.activation(out=w("kbg"), in_=ktok[:, n, :], func=AF.Copy, scale=gs("bkg")), [Rktok, Rgs], [r("kbg")])
                G(lambda e: e.tensor_scalar(out=w("kg"), in0=ktok[:, n, :], scalar1=gs("ekg"), scalar2=None, op0=ALU.mult), [Rktok, Rgs], [r("kg")])
                G(lambda e: e.tensor_scalar(out=w("vb"), in0=vtok[:, n, :], scalar1=gs("beta"), scalar2=None, op0=ALU.mult), [Rvtok, Rgs], [r("vb")])
                yield
                P(lambda e: e.matmul(qv(bk, 2), lhsT=ones, rhs=w("dgc"), start=True, stop=False), [Rc, r("dgc")], [Rk])
                P(lambda e: e.matmul(qv(bk, 2), lhsT=w("dng"), rhs=ones, start=False, stop=True), [Rc, r("dng")], [Rk])
                yield
                V(lambda e: e.scalar_tensor_tensor(out=w("m2"), in0=qv(bk, 2), scalar=0.0, in1=pmS[d][:], op0=ALU.max, op1=ALU.add), [Rk, Rmk], [r("m2")])
                V(lambda e: e.scalar_tensor_tensor(out=w("m1"), in0=qv(bk, 2), scalar=0.0, in1=nmT[d][:], op0=ALU.min, op1=ALU.add), [Rk, Rmk], [r("m1")])
                yield
                A(lambda e: e.activation(out=w("E2m"), in_=w("m2"), func=AF.Exp, scale=-1.0), [r("m2")], [r("E2m")])
                A(lambda e: e.activation(out=w("E1m"), in_=w("m1"), func=AF.Exp), [r("m1")], [r("E1m")])
                yield
                V(lambda e: e.scalar_tensor_tensor(out=w("N"), in0=qv(bk, 0), scalar=gs("negbeta"), in1=w("E2m"), op0=ALU.mult, op1=ALU.mult),
                  [Rk, Rgs, r("E2m")], [r("N")])
                V(lambda e: e.tensor_tensor(out=w("attnT"), in0=qv(bk, 1), in1=w("E1m"), op=ALU.mult), [Rk, r("E1m")], [r("attnT")])
                yield
                P(lambda e: e.matmul(qv(bk, 3), lhsT=w("N"), rhs=identI, start=True, stop=True), [r("N"), Rc], [Rk])
                yield
                A(lambda e: e.activation(out=w("Nt"), in_=qv(bk, 3), func=AF.Copy), [Rk], [r("Nt")])
                yield
                V(lambda e: e.tensor_tensor(out=w("Qa"), in0=w("Nt"), in1=ident, op=ALU.add), [r("Nt"), Rc], [r("Qa")])
                cn, cnt_, qa, qb = "N", "Nt", "Qa", "Qb"
                for k in range(1, 7):
                    nn, nnt = ("N2", "Nt2") if cn == "N" else ("N", "Nt")
                    P(lambda e: e.matmul(qv(bk, 0), lhsT=w(cnt_), rhs=w(cn), start=True, stop=True), [r(cnt_), r(cn)], [Rk])
                    if k < 6:
                        P(lambda e: e.matmul(qv(bk, 1), lhsT=w(cn), rhs=w(cnt_), start=True, stop=True), [r(cnt_), r(cn)], [Rk])
                    yield
                    A(lambda e: e.activation(out=w(nn), in_=qv(bk, 0), func=AF.Copy), [Rk], [r(nn)])
                    if k < 6:
                        V(lambda e: e.tensor_copy(out=w(nnt), in_=qv(bk, 1)), [Rk], [r(nnt)])
                    yield
                    P(lambda e: e.matmul(qv(bk, 2), lhsT=w(nn), rhs=w(qa), start=True, stop=True), [r(nn), r(qa)], [Rk])
                    yield
                    if k < 6:
                        V(lambda e: e.scalar_tensor_tensor(out=w(qb), in0=qv(bk, 2), scalar=1.0, in1=w(qa), op0=ALU.mult, op1=ALU.add), [Rk, r(qa)], [r(qb)])
                    else:
                        V(lambda e: e.scalar_tensor_tensor(out=w("TT"), in0=qv(bk, 2), scalar=1.0, in1=w(qa), op0=ALU.mult, op1=ALU.add), [Rk, r(qa)], [r("TT")])
                    cn, cnt_ = nn, nnt
                    qa, qb = qb, qa
                yield
                P(lambda e: e.matmul(qv(bk, 3), lhsT=w("TT"), rhs=w("vb"), start=True, stop=True), [r("TT"), r("vb")], [Rk])
                P(lambda e: e.matmul(qv(bk, 1), lhsT=w("kbg"), rhs=w("TT"), start=True, stop=True), [r("TT"), r("kbg")], [Rk])
                yield
                A(lambda e: e.activation(out=w("u"), in_=qv(bk, 3), func=AF.Copy), [Rk], [r("u")])
                A(lambda e: e.activation(out=w("wT"), in_=qv(bk, 1), func=AF.Copy), [Rk], [r("wT")])

            def seq(n, d, sl_):
                w = lambda nm: (W[(nm, d, "h", sl_)] if nm in HO else W[(nm, d)])[:]
                r = lambda nm: RW[(nm, d, "h", sl_)] if nm in HO else RW[(nm, d)]
                gs = lambda nm: gsc[(nm, d)][:, n:n + 1]
                bS = 4 + d
                tk = slice(n * 128, (n + 1) * 128)
                P(lambda e: e.matmul(qv(bS, 0), lhsT=w("wT"), rhs=w("Sb"), start=True, stop=True), [r("wT"), r("Sb")], [Rq[bS][0]])
                if n >= 2:
                    P(lambda e: e.matmul(qv(bS, 2), lhsT=qT[:, tk], rhs=w("Sb"), start=True, stop=True), [RqT, r("Sb")], [Rq[bS][2]])
                yield
                V(lambda e: e.tensor_tensor(out=w("vnew"), in0=w("u"), in1=qv(bS, 0), op=ALU.subtract), [r("u"), Rq[bS][0]], [r("vnew")])
                if n >= 2:
                    A(lambda e: e.activation(out=w("otmp"), in_=qv(bS, 2), func=AF.Copy, scale=gs("eg")), [Rq[bS][2], Rgs], [r("otmp")])
                yield
                P(lambda e: e.matmul(qv(bS, 1), lhsT=w("kg"), rhs=w("vnew"), start=True, stop=True), [r("kg"), r("vnew")], [Rq[bS][1]])
                if n >= 2:
                    P(lambda e: e.matmul(qv(bS, 3), lhsT=w("attnT"), rhs=w("vnew"), start=True, stop=True), [r("attnT"), r("vnew")], [Rq[bS][3]])
                yield
                V(lambda e: e.scalar_tensor_tensor(out=w("S"), in0=w("S"), scalar=gsc[("egl", d)][:, n:n + 1], in1=qv(bS, 1), op0=ALU.mult, op1=ALU.add),
                  [r("S"), Rgs, Rq[bS][1]], [r("S")])
                yield
                A(lambda e: e.activation(out=w("Sb"), in_=w("S"), func=AF.Copy), [r("S")], [r("Sb")])
                if n >= 2:
                    lt = n - 2
                    if lt not in touched:
                        touched.add(lt)
                        V(lambda e: e.tensor_tensor(out=oacc[:, lt, :], in0=w("otmp"), in1=qv(bS, 3), op=ALU.add), [r("otmp"), Rq[bS][3]], [Roacc[lt]])
                    else:
                        V(lambda e: e.tensor_tensor(out=w("otmp2"), in0=w("otmp"), in1=qv(bS, 3), op=ALU.add), [r("otmp"), Rq[bS][3]], [r("otmp2")])
                        yield
                        G(lambda e: e.tensor_tensor(out=oacc[:, lt, :], in0=oacc[:, lt, :], in1=w("otmp2"), op=ALU.add), [r("otmp2"), Roacc[lt]], [Roacc[lt]])

            order = [[0, 1] + list(range(2, NT)), [1, 0] + list(range(NT - 1, 1, -1))]
            import os as _os
            nsteps = NT if stage >= 3 else int(_os.environ.get('KNSTEPS', '6'))
            nprep = [0, 0]; prep_act = [[], []]; prep_done = [set(), set()]
            nseq = [0, 0]; seq_act = [None, None]
            Rwarm = Reg()

            def warm(nmm):
                for _ in range(nmm):
                    P(lambda e: e.matmul(bank[6][:, 0:128], lhsT=identb[:], rhs=qT[:, 0:128], start=True, stop=True), [Rc, RqT], [Rwarm])

            last_warm = -1
            while nseq[0] < nsteps or nseq[1] < nsteps:
                if nseq[0] % 4 == 0 and nseq[0] != last_warm:
                    last_warm = nseq[0]
                    warm(64 if nseq[0] == 0 else 40)
                for d in range(2):
                    while (len(prep_act[d]) < NCTX and nprep[d] < nsteps and nprep[d] < nseq[d] + NSLOT
                           and all(p_[0] % NCTX != nprep[d] % NCTX for p_ in prep_act[d])):
                        s_n = nprep[d]
                        prep_act[d].append((s_n, prep(order[d][s_n], d, s_n % NCTX, s_n % NSLOT)))
                        nprep[d] += 1
                    if seq_act[d] is None and nseq[d] < nsteps and nseq[d] in prep_done[d]:
                        seq_act[d] = seq(order[d][nseq[d]], d, nseq[d] % NSLOT)
                for d in range(2):
                    if seq_act[d] is not None:
                        try:
                            next(seq_act[d])
                        except StopIteration:
                            seq_act[d] = None
                            nseq[d] += 1
                for d in range(2):
                    for p_ in list(prep_act[d]):
                        try:
                            next(p_[1])
                        except StopIteration:
                            prep_act[d].remove(p_)
                            prep_done[d].add(p_[0])
            if dbg:
                Rd = Reg()
                d_ = dout("dbg_oacc", [128, 64 * 128])
                fw.dma("sp", lambda e: e.dma_start(out=d_, in_=oacc[:].rearrange("p t c -> p (t c)")), Roacc, [Rd])
                for d in range(2):
                    d_ = dout("dbg_S%d" % d, [128, 128])
                    fw.dma("sp", lambda e: e.dma_start(out=d_, in_=W[("S", d)][:]), [RW[("S", d)]], [Rd])
                    d_ = dout("dbg_TT%d" % d, [128, 128], BF16)
                    fw.dma("sp", lambda e: e.dma_start(out=d_, in_=W[("TT", d)][:]), [RW[("TT", d)]], [Rd])
                fw.finish([Rd])
            fw.pop_scope()
            fw.pop_scope()
        if stage >= 4:
            yloc_c = [nc.dram_tensor("yloc%d" % k, [256, 2048], BF16).ap() for k in range(4)]
            ycat_d = nc.dram_tensor("ycat", [4 * 1024, 2048], BF16).ap()
            Ryloc = [Reg() for _ in range(4)]; Rycat = Reg()
            fw.push_scope()
            wsf = sb("wsf", [128, 128]); Rwsf = Reg()
            fw.dma("sp", lambda e: e.dma_start(out=wsf[:], in_=gmws_d), writes=[Rwsf])
            wsT = sb("wsT", [128, 128], BF16); RwsT = Reg()
            P(lambda e: e.matmul(bank[0][:, 0:128], lhsT=wsf[:], rhs=ident, start=True, stop=True), [Rwsf, Rc], [Rb[0]])
            V(lambda e: e.tensor_copy(out=wsT[:], in_=bank[0][:, 0:128]), [Rb[0]], [RwsT])
            yst = [sb("yst%d" % i, [128, 2, 512], BF16) for i in range(2)]; Ryst = [Reg(), Reg()]
            junk5 = sb("junk5", [128, 128]); Rj5 = Reg()
            st5 = [sb("st5_%d" % i, [128, 4]) for i in range(2)]; Rst5 = [Reg(), Reg()]
            t1 = [sb("t1_%d" % i, [128, 128]) for i in range(2)]; Rt1 = [Reg(), Reg()]
            ybt = [sb("ybt%d" % i, [128, 128], BF16) for i in range(2)]; Rybt = [Reg(), Reg()]
            t2 = [sb("t2_%d" % i, [128, 128]) for i in range(2)]; Rt2 = [Reg(), Reg()]
            for lt in range(64):
                s_ = lt % 2
                blk = lt // 4
                ys = yst[blk % 2]; Rys = Ryst[blk % 2]
                cs = slice((lt % 4) * 128, (lt % 4 + 1) * 128)
                A(lambda e: e.activation(out=junk5[:], in_=oacc[:, lt, :], func=AF.Square, accum_out=st5[s_][:, 0:1]), [Roacc[lt]], [Rj5, Rst5[s_]])
                A(lambda e: e.activation(out=st5[s_][:, 1:2], in_=st5[s_][:, 0:1], func=AF.Sqrt, scale=1.0 / 128, bias=epst[:, 0:1]), [Rst5[s_], Rc], [Rst5[s_]])
                V(lambda e: e.reciprocal(out=st5[s_][:, 2:3], in_=st5[s_][:, 1:2]), [Rst5[s_]], [Rst5[s_]])
                V(lambda e: e.scalar_tensor_tensor(out=t1[s_][:], in0=oacc[:, lt, :], scalar=st5[s_][:, 2:3], in1=srow[:, 4:132], op0=ALU.mult, op1=ALU.mult),
                  [Roacc[lt], Rst5[s_], Rsrow], [Rt1[s_]])
                V(lambda e: e.tensor_tensor(out=ybt[s_][:], in0=t1[s_][:], in1=zs[:, lt, :], op=ALU.mult), [Rt1[s_], Rzs], [Rybt[s_]])
                bkT = 1 + s_
                pTb = bank[bkT][:, :].bitcast(BF16)
                P(lambda e: e.transpose(out=pTb[:, 0:128], in_=ybt[s_][:], identity=identb[:]), [Rybt[s_], Rc], [Rb[bkT]])
                A(lambda e: e.activation(out=ys[:, 1, cs], in_=pTb[:, 0:128], func=AF.Copy), [Rb[bkT]], [Rys])
                bkS = 3 + s_
                P(lambda e: e.matmul(bank[bkS][:, 0:128], lhsT=vgn[:, lt, :], rhs=wsT[:], start=True, stop=True), [Rvgn, RwsT], [Rb[bkS]])
                V(lambda e: e.tensor_tensor(out=t2[s_][:], in0=bank[bkS][:, 0:128], in1=srow[:, 260:388], op=ALU.add), [Rb[bkS], Rsrow], [Rt2[s_]])
                V(lambda e: e.tensor_tensor(out=ys[:, 0, cs], in0=t2[s_][:], in1=uT[:, lt * 128:(lt + 1) * 128], op=ALU.mult), [Rt2[s_], RuT], [Rys])
                if lt % 4 == 3:
                    ck = blk // 4
                    c0 = (blk % 4) * 512
                    fw.dma("sp", lambda e: e.dma_start(out=yloc_c[ck][:, c0:c0 + 512].rearrange("(a p) n -> p a n", p=128), in_=ys[:]), [Rys], [Ryloc[ck]])
                    if blk % 4 == 3:
                        fw.cc(lambda e: e.collective_compute("AllGather", ALU.bypass, replica_groups=[[0, 1, 2, 3], [4, 5, 6, 7]],
                                                             ins=[yloc_c[ck]], outs=[ycat_d[ck * 1024:(ck + 1) * 1024, :]]), [Ryloc[ck]], [Rycat])
            fw.pop_scope()
            if dbg:
                Rd = Reg()
                for ck in range(4):
                    d_ = dout("dbg_yloc%d" % ck, [256, 2048], BF16)
                    fw.dma("sp", lambda e: e.dma_start(out=d_, in_=yloc_c[ck]), [Ryloc[ck]], [Rd])
                d_ = dout("dbg_ycat", [4096, 2048], BF16)
                fw.dma("sp", lambda e: e.dma_start(out=d_, in_=ycat_d), [Rycat], [Rd])
                fw.finish([Rd])
        fw.pop_scope()
        if stage >= 5:
            h_d = nc.dram_tensor("h_scr", [2048, D], F32).ap()
            fin_d = nc.dram_tensor("fin_scr", [2048 + 128, D], BF16).ap()
            affloc_d = nc.dram_tensor("affloc", [128, 256], F32).ap()
            affall_d = nc.dram_tensor("affall", [512, 256], F32).ap()
            Rh = Reg(); Rfin = Reg(); Raffloc = Reg(); Raffall = Reg()
            mrow = sb("mrow", [128, 6 * D]); Rmrow = Reg()
            fw.dma("sp", lambda e: e.dma_start(out=mrow[:], in_=modrow_d), [Rmodrow], [Rmrow])
            affsb = sb("affsb", [128, 16, NE]); Raffsb = Reg()
            fw.push_scope()
            yidx = sb("yidx", [128, 8], I32); Ryidx = Reg()
            fw.dma("sp", lambda e: e.dma_start(out=yidx[:], in_=yidx_d), writes=[Ryidx])
            wo = sb("wo", [128, 8, D], BF16); Rwo = Reg()
            fw.dma("pool", lambda e: e.dma_start(out=wo[:], in_=wout_d.rearrange("(j k) n -> k j n", k=128)), writes=[Rwo])
            wr = sb("wr", [128, 8, NE]); Rwr = Reg()
            fw.dma("sp", lambda e: e.dma_start(out=wr[:], in_=wr_d.rearrange("(j k) n -> k j n", k=128)), writes=[Rwr])
            brow = sb("brow", [1, NE]); Rbrow = Reg()
            fw.dma("sp", lambda e: e.dma_start(out=brow[:], in_=br_d), writes=[Rbrow])
            ysb = sb("ysb", [128, 8, 2048], BF16); Rysb = [Reg() for _ in range(8)]
            for j in range(8):
                fw.dma("pool", lambda e: e.indirect_dma_start(out=ysb[:, j, :], out_offset=None, in_=ycat_d[:, :],
                                                              in_offset=bass.IndirectOffsetOnAxis(ap=yidx[:, j:j + 1], axis=0)),
                       [Rycat, Ryidx], [Rysb[j]])
            xo = [sb("xo%d" % i, [128, D]) for i in range(2)]; Rxo = [Reg(), Reg()]
            hsb = [sb("hsb%d" % i, [128, D]) for i in range(2)]; Rhsb = [Reg(), Reg()]
            fsb = [sb("fsb%d" % i, [128, D]) for i in range(2)]; Rfsb = [Reg(), Reg()]
            fbf = [sb("fbf%d" % i, [128, D], BF16) for i in range(2)]; Rfbf = [Reg(), Reg()]
            fT = [sb("fT%d" % i, [128, 8, 128]) for i in range(2)]; RfT = [Reg(), Reg()]
            jb = sb("jb", [128, D]); Rjb = Reg()
            stb = [sb("stb%d" % i, [128, 8]) for i in range(2)]; Rstb = [Reg(), Reg()]
            lg = [sb("lg%d" % i, [128, NE]) for i in range(2)]; Rlg = [Reg(), Reg()]
            def b_tile(ti):
                s_ = ti % 2
                b0, b1 = (0, 1) if s_ == 0 else (2, 3)
                fw.dma("sp", lambda e: e.dma_start(out=xo[s_][:], in_=xown_d[ti * 128:(ti + 1) * 128, :]), writes=[Rxo[s_]])
                for hf, bk in ((0, b0), (1, b1)):
                    for j in range(8):
                        P(lambda e: e.matmul(bank[bk][:, :], lhsT=ysb[:, j, ti * 128:(ti + 1) * 128], rhs=wo[:, j, hf * 512:(hf + 1) * 512],
                                             start=(j == 0), stop=(j == 7)), [Rysb[j], Rwo], [Rb[bk]])
                yield
                for hf, bk in ((0, b0), (1, b1)):
                    cs = slice(hf * 512, (hf + 1) * 512)
                    V(lambda e: e.tensor_tensor(out=hsb[s_][:, cs], in0=bank[bk][:, :], in1=mrow[:, hf * 512:(hf + 1) * 512], op=ALU.mult),
                      [Rb[bk], Rmrow], [Rhsb[s_]])
                yield
                V(lambda e: e.tensor_tensor(out=hsb[s_][:], in0=hsb[s_][:], in1=xo[s_][:], op=ALU.add), [Rhsb[s_], Rxo[s_]], [Rhsb[s_]])
                yield
                fw.dma("sp", lambda e: e.dma_start(out=h_d[ti * 128:(ti + 1) * 128, :], in_=hsb[s_][:]), [Rhsb[s_]], [Rh])
                A(lambda e: e.activation(out=jb[:], in_=hsb[s_][:], func=AF.Square, accum_out=stb[s_][:, 0:1]), [Rhsb[s_]], [Rjb, Rstb[s_]])
                yield
                A(lambda e: e.activation(out=stb[s_][:, 1:2], in_=stb[s_][:, 0:1], func=AF.Sqrt, scale=1.0 / D, bias=epst[:, 0:1]), [Rstb[s_], Rc], [Rstb[s_]])
                yield
                V(lambda e: e.reciprocal(out=stb[s_][:, 2:3], in_=stb[s_][:, 1:2]), [Rstb[s_]], [Rstb[s_]])
                yield
                V(lambda e: e.scalar_tensor_tensor(out=fsb[s_][:], in0=hsb[s_][:], scalar=stb[s_][:, 2:3], in1=mrow[:, 2 * D:3 * D], op0=ALU.mult, op1=ALU.mult),
                  [Rhsb[s_], Rstb[s_], Rmrow], [Rfsb[s_]])
                yield
                V(lambda e: e.tensor_tensor(out=fsb[s_][:], in0=fsb[s_][:], in1=mrow[:, D:2 * D], op=ALU.add), [Rfsb[s_], Rmrow], [Rfsb[s_]])
                yield
                A(lambda e: e.activation(out=fbf[s_][:], in_=fsb[s_][:], func=AF.Copy), [Rfsb[s_]], [Rfbf[s_]])
                for j in range(8):
                    bk = b0 if j < 4 else b1
                    q_ = j % 4
                    P(lambda e: e.matmul(bank[bk][:, q_ * 128:(q_ + 1) * 128], lhsT=fsb[s_][:, j * 128:(j + 1) * 128], rhs=ident, start=True, stop=True),
                      [Rfsb[s_], Rc], [Rb[bk]])
                yield
                fw.dma("sp", lambda e: e.dma_start(out=fin_d[ti * 128:(ti + 1) * 128, :], in_=fbf[s_][:]), [Rfbf[s_]], [Rfin])
                V(lambda e: e.tensor_copy(out=fT[s_][:, 0:4, :], in_=bank[b0][:, :].rearrange("p (q c) -> p q c", c=128)), [Rb[b0]], [RfT[s_]])
                A(lambda e: e.activation(out=fT[s_][:, 4:8, :], in_=bank[b1][:, :].rearrange("p (q c) -> p q c", c=128), func=AF.Copy), [Rb[b1]], [RfT[s_]])
                yield
                for j in range(8):
                    P(lambda e: e.matmul(bank[b0][:, 0:NE], lhsT=fT[s_][:, j, :], rhs=wr[:, j, :], start=(j == 0), stop=False), [RfT[s_], Rwr], [Rb[b0]])
                P(lambda e: e.matmul(bank[b0][:, 0:NE], lhsT=ones[0:1, :], rhs=brow[0:1, :], start=False, stop=True), [Rc, Rbrow], [Rb[b0]])
                yield
                V(lambda e: e.tensor_copy(out=lg[s_][:], in_=bank[b0][:, 0:NE]), [Rb[b0]], [Rlg[s_]])
                yield
                V(lambda e: e.tensor_reduce(out=stb[s_][:, 3:4], in_=lg[s_][:], axis=AX.X, op=ALU.max), [Rlg[s_]], [Rstb[s_]])
                yield
                V(lambda e: e.tensor_scalar(out=stb[s_][:, 4:5], in0=stb[s_][:, 3:4], scalar1=-1.0, scalar2=None, op0=ALU.mult), [Rstb[s_]], [Rstb[s_]])
                yield
                A(lambda e: e.activation(out=lg[s_][:], in_=lg[s_][:], func=AF.Exp, bias=stb[s_][:, 4:5], accum_out=stb[s_][:, 5:6]), [Rlg[s_], Rstb[s_]], [Rlg[s_], Rstb[s_]])
                yield
                V(lambda e: e.reciprocal(out=stb[s_][:, 6:7], in_=stb[s_][:, 5:6]), [Rstb[s_]], [Rstb[s_]])
                yield
                V(lambda e: e.tensor_scalar(out=affsb[:, ti, :], in0=lg[s_][:], scalar1=stb[s_][:, 6:7], scalar2=None, op0=ALU.mult), [Rlg[s_], Rstb[s_]], [Raffsb])
            for t2 in range(0, 16, 2):
                run_rr([b_tile(t2), b_tile(t2 + 1)])
            fw.dma("sp", lambda e: e.dma_start(out=affloc_d, in_=affsb[:].rearrange("p t e -> p (t e)")), [Raffsb], [Raffloc])
            fw.pop_scope()
            fw.cc(lambda e: e.collective_compute("AllGather", ALU.bypass, replica_groups=[[0, 1, 2, 3], [4, 5, 6, 7]],
                                                 ins=[affloc_d], outs=[affall_d]), [Raffloc], [Raffall])
            if dbg:
                Rd = Reg()
                d_ = dout("dbg_h", [2048, D])
                fw.dma("sp", lambda e: e.dma_start(out=d_, in_=h_d), [Rh], [Rd])
                d_ = dout("dbg_fin", [2048, D], BF16)
                fw.dma("sp", lambda e: e.dma_start(out=d_, in_=fin_d[0:2048, :]), [Rfin], [Rd])
                d_ = dout("dbg_affall", [512, 256])
                fw.dma("sp", lambda e: e.dma_start(out=d_, in_=affall_d), [Raffall], [Rd])
                fw.finish([Rd])
        if stage <= 5:
            Ro = Reg()
            fw.dma("sp", lambda e: e.dma_start(out=out_d[0:128, 0:32], in_=fm[:].rearrange("k a n -> k (a n)")), [Rfm], [Ro])
            fw.finish([Ro])
            return nc, list(dbg_d.keys())
        NIT = 30
        rc = sb("rc", [128, 16 + SLOTS]); Rrc = Reg()
        fw.dma("sp", lambda e: e.dma_start(out=rc[:], in_=rc_d), writes=[Rrc])
        idxi = sb("idxi", [128, NE, 4], I32); Ridx = Reg()
        gate = sb("gate", [128, NE, 4]); Rgate = Reg()
        wgs = [sb("wgs%d" % i, [128, 8, D], BF16) for i in range(2)]; Rwg = [Reg(), Reg()]
        wus = [sb("wus%d" % i, [128, 8, D], BF16) for i in range(2)]; Rwu = [Reg(), Reg()]
        wds = [sb("wds%d" % i, [128, 8, D], BF16) for i in range(2)]; Rwd = [Reg(), Reg()]
        def loads(ex):
            s_ = ex % 2
            fw.dma("pool", lambda e: e.dma_start(out=wgs[s_][:], in_=wg_d[ex].rearrange("(j k) n -> k j n", k=128)), [], [Rwg[s_]])
            fw.dma("pool", lambda e: e.dma_start(out=wus[s_][:], in_=wu_d[ex].rearrange("(j k) n -> k j n", k=128)), [], [Rwu[s_]])
            fw.dma("pool", lambda e: e.dma_start(out=wds[s_][:], in_=wd_d[ex].rearrange("(j k) n -> k j n", k=128)), [], [Rwd[s_]])

        loads(0)
        loads(1)
        fw.push_scope()
        Aall = sb("Aall", [128, 4, 256]); RAall = Reg()
        fw.dma("sp", lambda e: e.dma_start(out=Aall[:], in_=affall_d.rearrange("(r p) n -> p r n", p=128)), [Raffall], [RAall])
        lo = sb("lo", [128, NE]); hi = sb("hi", [128, NE]); mid = sb("mid", [128, NE]); Rth = Reg()
        cmpt = sb("cmpt", [128, 1024]); Rcmp = Reg()
        cntp = sb("cntp", [128, NE]); Rcnt = Reg()
        ge = sb("ge", [128, NE]); dl = sb("dl", [128, NE]); dh = sb("dh", [128, NE])
        V(lambda e: e.memset(lo[:], 0.0), [], [Rth])
        V(lambda e: e.memset(hi[:], 1.0), [], [Rth])
        Aview = Aall[:].rearrange("p r (t e) -> p (r t) e", e=NE)
        for it in range(NIT):
            hw_ = 0.5 ** (it + 1)
            V(lambda e: e.tensor_scalar(out=mid[:], in0=lo[:], scalar1=hw_, scalar2=None, op0=ALU.add), [Rth], [Rth])
            V(lambda e: e.tensor_tensor(out=cmpt[:].rearrange("p (n e) -> p n e", e=NE), in0=Aview,
                                        in1=mid[:].unsqueeze(1).to_broadcast([128, 64, NE]), op=ALU.is_ge), [RAall, Rth], [Rcmp])
            V(lambda e: e.tensor_reduce(out=cntp[:], in_=cmpt[:].rearrange("p (n e) -> p e n", e=NE), axis=AX.X, op=ALU.add), [Rcmp], [Rcnt])
            P(lambda e: e.matmul(bank[0][:, 0:NE], lhsT=ones, rhs=cntp[:], start=True, stop=True), [Rcnt, Rc], [Rb[0]])
            V(lambda e: e.tensor_scalar(out=ge[:], in0=bank[0][:, 0:NE], scalar1=CAP - 0.5, scalar2=None, op0=ALU.is_ge), [Rb[0]], [Rth])
            V(lambda e: e.scalar_tensor_tensor(out=lo[:], in0=ge[:], scalar=hw_, in1=lo[:], op0=ALU.mult, op1=ALU.add), [Rth], [Rth])
        sel = sb("sel", [128, 256]); Rsel = Reg()
        V(lambda e: e.tensor_tensor(out=sel[:].rearrange("p (t e) -> p t e", e=NE), in0=affsb[:], in1=lo[:].unsqueeze(1).to_broadcast([128, 16, NE]), op=ALU.is_ge),
          [Raffsb, Rth], [Rsel])
        P(lambda e: e.matmul(bank[1][:, 0:256], lhsT=U_st, rhs=sel[:], start=True, stop=True), [Rsel, Rc], [Rb[1]])
        P(lambda e: e.matmul(bank[2][:, 0:256], lhsT=ones, rhs=sel[:], start=True, stop=True), [Rsel, Rc], [Rb[2]])
        tot = sb("tot", [128, 16, NE]); pre = sb("pre", [128, 16, NE]); Rpre_ = Reg()
        V(lambda e: e.tensor_copy(out=tot[:].rearrange("p t e -> p (t e)"), in_=bank[2][:, 0:256]), [Rb[2]], [Rpre_])
        V(lambda e: e.memset(pre[:, 0, :], 0.0), [], [Rpre_])
        for ti in range(1, 16):
            V(lambda e: e.tensor_tensor(out=pre[:, ti, :], in0=pre[:, ti - 1, :], in1=tot[:, ti - 1, :], op=ALU.add), [Rpre_], [Rpre_])
        rank = sb("rank", [128, 256]); Rrank = Reg()
        V(lambda e: e.tensor_tensor(out=rank[:], in0=bank[1][:, 0:256], in1=pre[:].rearrange("p t e -> p (t e)"), op=ALU.add), [Rb[1], Rpre_], [Rrank])
        Rall = sb("Rall", [128, 16, NE, 4]); RRall = Reg()
        V(lambda e: e.memset(Rall[:].rearrange("p t e c -> p (t e c)"), 1.0), [], [RRall])
        V(lambda e: e.tensor_copy(out=Rall[:, :, :, 0], in_=rc[:, 0:16].unsqueeze(2).to_broadcast([128, 16, NE])), [Rrc], [RRall])
        V(lambda e: e.tensor_copy(out=Rall[:, :, :, 1], in_=affsb[:]), [Raffsb], [RRall])
        oh = [sb("oh%d" % i, [128, 16, SLOTS]) for i in range(2)]; Roh = [Reg(), Reg()]
        sinfo = sb("sinfo", [128, 3, 4]); Rsinfo = Reg()
        itmp = sb("itmp", [128, 3]); Ritmp = Reg()
        V(lambda e: e.memset(gate[:].rearrange("p e c -> p (e c)"), 0.0), [], [Rgate])
        def oh_ops(ex):
            o_ = ex % 2
            for ti in range(16):
                col = ti * NE + ex
                V(lambda e: e.tensor_scalar(out=oh[o_][:, ti, :], in0=rc[:, 16:16 + SLOTS], scalar1=rank[:, col:col + 1], scalar2=sel[:, col:col + 1],
                                            op0=ALU.is_equal, op1=ALU.mult), [Rrc, Rrank, Rsel], [Roh[o_]])

        def mm_ops(ex):
            bkI = 3 + (ex % 2)
            o_ = ex % 2
            for c in range(3):
                M = 128 if c < 2 else SLOTS - 256
                for ti in range(16):
                    P(lambda e: e.matmul(bank[bkI][0:M, c * 128:c * 128 + 4], lhsT=oh[o_][:, ti, c * 128:c * 128 + M], rhs=Rall[:, ti, ex, :],
                                         start=(ti == 0), stop=(ti == 15)), [Roh[o_], RRall], [Rb[bkI]])

        def sinfo_ops(ex):
            bkI = 3 + (ex % 2)
            q_ = ex % 2
            V(lambda e: e.memset(sinfo2[q_][:].rearrange("p c k -> p (c k)"), 0.0), [], [Rsinfo2[q_]])
            V(lambda e: e.tensor_copy(out=sinfo2[q_][:, 0, :], in_=bank[bkI][:, 0:4]), [Rb[bkI]], [Rsinfo2[q_]])
            V(lambda e: e.tensor_copy(out=sinfo2[q_][:, 1, :], in_=bank[bkI][:, 128:132]), [Rb[bkI]], [Rsinfo2[q_]])
            V(lambda e: e.tensor_copy(out=sinfo2[q_][0:64, 2, :], in_=bank[bkI][0:64, 256:260]), [Rb[bkI]], [Rsinfo2[q_]])
            V(lambda e: e.tensor_scalar(out=itmp2[q_][:], in0=sinfo2[q_][:, :, 2], scalar1=-2048.0, scalar2=2048.0, op0=ALU.mult, op1=ALU.add), [Rsinfo2[q_]], [Ritmp2[q_]])
            V(lambda e: e.tensor_tensor(out=itmp2[q_][:], in0=itmp2[q_][:], in1=sinfo2[q_][:, :, 0], op=ALU.add), [Rsinfo2[q_], Ritmp2[q_]], [Ritmp2[q_]])
            V(lambda e: e.tensor_copy(out=idxi[:, ex, 0:3], in_=itmp2[q_][:]), [Ritmp2[q_]], [Ridx])
            V(lambda e: e.tensor_copy(out=gate[:, ex, 0:3], in_=sinfo2[q_][:, :, 1]), [Rsinfo2[q_]], [Rgate])

        sinfo2 = [sinfo, sb("sinfo_b", [128, 3, 4])]; Rsinfo2 = [Rsinfo, Reg()]
        itmp2 = [itmp, sb("itmp_b", [128, 3])]; Ritmp2 = [Ritmp, Reg()]
        oh_ops(0)
        for ex in range(NE):
            if ex + 1 < NE:
                oh_ops(ex + 1)
            mm_ops(ex)
            sinfo_ops(ex)
        if dbg:
            Rd = Reg()
            d_ = dout("dbg_lo", [128, NE])
            fw.dma("sp", lambda e: e.dma_start(out=d_, in_=lo[:]), [Rth], [Rd])
            d_ = dout("dbg_hi", [128, NE])
            fw.dma("sp", lambda e: e.dma_start(out=d_, in_=hi[:]), [Rth], [Rd])
            d_ = dout("dbg_rank", [128, 256])
            fw.dma("sp", lambda e: e.dma_start(out=d_, in_=rank[:]), [Rrank], [Rd])
            d_ = dout("dbg_sel", [128, 256])
            fw.dma("sp", lambda e: e.dma_start(out=d_, in_=sel[:]), [Rsel], [Rd])
            d_ = dout("dbg_idxi", [128, NE * 4], I32)
            fw.dma("sp", lambda e: e.dma_start(out=d_, in_=idxi[:].rearrange("p e c -> p (e c)")), [Ridx], [Rd])
            d_ = dout("dbg_sinfo", [128, 12])
            fw.dma("sp", lambda e: e.dma_start(out=d_, in_=sinfo[:].rearrange("p c k -> p (c k)")), [Rsinfo], [Rd])
            d_ = dout("dbg_Rall", [128, 16 * NE * 4])
            fw.dma("sp", lambda e: e.dma_start(out=d_, in_=Rall[:].rearrange("p t e c -> p (t e c)")), [RRall], [Rd])
            d_ = dout("dbg_gate", [128, NE * 4])
            fw.dma("sp", lambda e: e.dma_start(out=d_, in_=gate[:].rearrange("p e c -> p (e c)")), [Rgate], [Rd])
            fw.finish([Rd])
        fw.pop_scope()
        if stage <= 6:
            Ro = Reg()
            fw.dma("sp", lambda e: e.dma_start(out=out_d[0:128, 0:32], in_=fm[:].rearrange("k a n -> k (a n)")), [Rfm], [Ro])
            fw.finish([Ro])
            return nc, list(dbg_d.keys())
        acc_d = nc.dram_tensor("acc_scr", [2048 + 128, D], F32).ap()
        Racc = Reg()
        fw.push_scope()
        xe = [sb("xe%d" % i, [128, 3, D], BF16) for i in range(2)]; Rxe = [Reg(), Reg()]
        xeT = [sb("xeT%d" % i, [128, 8, SLOTS], BF16) for i in range(2)]; RxeT = [Reg(), Reg()]
        hid = [sb("hid%d" % i, [128, 8, SLOTS], BF16) for i in range(2)]; Rhid = [Reg(), Reg()]
        sg = [sb("sg%d" % i, [128, SLOTS]) for i in range(2)]; Rsg = [Reg(), Reg()]
        ye0_ = sb("ye0", [128, 3, D]); Rye0_ = Reg()
        ye = [ye0_, ye0_]; Rye = [Rye0_, Rye0_]
        wdf = sb("wdf", [128, 8, D]); Rwdf = Reg()

        def loads_gu(ex):
            s_ = ex % 2
            fw.dma("pool", lambda e: e.dma_start(out=wgs[s_][:], in_=wg_d[ex].rearrange("(j k) n -> k j n", k=128)), [], [Rwg[s_]])
            fw.dma("pool", lambda e: e.dma_start(out=wus[s_][:], in_=wu_d[ex].rearrange("(j k) n -> k j n", k=128)), [], [Rwu[s_]])

        def dma_down(ex):
            fw.dma("sp", lambda e: e.dma_start(out=wdf[:], in_=wd_d[ex].rearrange("(j k) n -> k j n", k=128)), [], [Rwdf])

        def cast_down(ex):
            s_ = ex % 2
            A(lambda e: e.activation(out=wds[s_][:, 0:4, :], in_=wdf[:, 0:4, :], func=AF.Copy), [Rwdf], [Rwd[s_]])
            V(lambda e: e.tensor_copy(out=wds[s_][:, 4:8, :], in_=wdf[:, 4:8, :]), [Rwdf], [Rwd[s_]])

        for i in range(2):
            V(lambda e: e.memset(xe[i][:].rearrange("p c n -> p (c n)"), 0.0), [], [Rxe[i]])
        V(lambda e: e.memset(ye[0][:].rearrange("p c n -> p (c n)"), 0.0), [], [Rye[0]])
        for ti in range(17):
            fw.dma("sp", lambda e: e.dma_start(out=acc_d[ti * 128:(ti + 1) * 128, :], in_=ye[0][:, 0, :]), [Rye[0]], [Racc])
        fw.dma("sp", lambda e: e.dma_start(out=fin_d[2048:2176, :], in_=xe[0][:, 0, :]), [Rxe[0]], [Rfin])
        CH = [(0, 128), (1, 128), (2, SLOTS - 256)]

        def gather(ex):
            s_ = ex % 2
            for c, M in CH:
                fw.dma("pool", lambda e: e.indirect_dma_start(out=xe[s_][0:M, c, :], out_offset=None, in_=fin_d[:, :],
                                                              in_offset=bass.IndirectOffsetOnAxis(ap=idxi[0:M, ex, c:c + 1], axis=0)),
                       [Rfin, Ridx], [Rxe[s_]])

        def compute(ex):
            s_ = ex % 2
            for c, M in CH:
                bk = c % 2
                pTb = bank[bk][:, :].bitcast(BF16)
                for j in range(8):
                    P(lambda e: e.transpose(out=pTb[:, j * 128:j * 128 + M], in_=xe[s_][0:M, c, j * 128:(j + 1) * 128], identity=identb[0:M, 0:M]),
                      [Rxe[s_], Rc], [Rb[bk]])
                src = pTb.rearrange("p (j m) -> p j m", m=128)[:, :, 0:M]
                if c % 2 == 0:
                    V(lambda e: e.tensor_copy(out=xeT[s_][:, :, c * 128:c * 128 + M], in_=src), [Rb[bk]], [RxeT[s_]])
                else:
                    A(lambda e: e.activation(out=xeT[s_][:, :, c * 128:c * 128 + M], in_=src, func=AF.Copy), [Rb[bk]], [RxeT[s_]])
            for f in range(8):
                bg = 2 + (f % 2); bu = 4 + (f % 2); q_ = f % 2
                for j in range(8):
                    P(lambda e: e.matmul(bank[bg][:, 0:SLOTS], lhsT=wgs[s_][:, j, f * 128:(f + 1) * 128], rhs=xeT[s_][:, j, :], start=(j == 0), stop=(j == 7)),
                      [Rwg[s_], RxeT[s_]], [Rb[bg]])
                for j in range(8):
                    P(lambda e: e.matmul(bank[bu][:, 0:SLOTS], lhsT=wus[s_][:, j, f * 128:(f + 1) * 128], rhs=xeT[s_][:, j, :], start=(j == 0), stop=(j == 7)),
                      [Rwu[s_], RxeT[s_]], [Rb[bu]])
                A(lambda e: e.activation(out=sg[q_][:], in_=bank[bg][:, 0:SLOTS], func=AF.Silu), [Rb[bg]], [Rsg[q_]])
                V(lambda e: e.tensor_tensor(out=hid[s_][:, f, :], in0=bank[bu][:, 0:SLOTS], in1=sg[q_][:], op=ALU.mult), [Rb[bu], Rsg[q_]], [Rhid[s_]])
            for c, M in CH:
                for hf in range(2):
                    bd = 6 + ((c * 2 + hf) % 2)
                    for f in range(8):
                        P(lambda e: e.matmul(bank[bd][0:M, :], lhsT=hid[s_][:, f, c * 128:c * 128 + M], rhs=wds[s_][:, f, hf * 512:(hf + 1) * 512],
                                             start=(f == 0), stop=(f == 7)), [Rhid[s_], Rwd[s_]], [Rb[bd]])
                    if hf == 0:
                        V(lambda e: e.tensor_scalar(out=ye[s_][0:M, c, 0:512], in0=bank[bd][0:M, :], scalar1=gate[0:M, ex, c:c + 1], scalar2=None, op0=ALU.mult),
                          [Rb[bd], Rgate], [Rye[s_]])
                    else:
                        A(lambda e: e.activation(out=ye[s_][0:M, c, 512:1024], in_=bank[bd][0:M, :], func=AF.Copy, scale=gate[0:M, ex, c:c + 1]),
                          [Rb[bd], Rgate], [Rye[s_]])

        def scatter(ex):
            s_ = ex % 2
            for c, M in CH:
                fw.dma("pool", lambda e: e.indirect_dma_start(out=acc_d[:, :], out_offset=bass.IndirectOffsetOnAxis(ap=idxi[0:M, ex, c:c + 1], axis=0),
                                                              in_=ye[s_][0:M, c, :], in_offset=None,
                                                              compute_op=ALU.add),
                       [Rye[s_], Ridx, Racc], [Racc])

        nexp = NE if stage >= 8 else 2
        gather(0)
        for ex in range(nexp):
            if ex + 1 < nexp:
                if ex + 1 >= 2:
                    loads_gu(ex + 1)
                    dma_down(ex + 1)
                gather(ex + 1)
            compute(ex)
            if 2 <= ex + 1 < nexp:
                cast_down(ex + 1)
            scatter(ex)
        fw.pop_scope()
        fw.push_scope()
        at = [sb("at%d" % i, [128, D]) for i in range(2)]; Rat = [Reg(), Reg()]
        ht = [sb("ht%d" % i, [128, D]) for i in range(2)]; Rht = [Reg(), Reg()]
        ot = [sb("ot%d" % i, [128, D]) for i in range(2)]; Rot = [Reg(), Reg()]
        jf = sb("jf", [128, D]); Rjf = Reg()
        stf = [sb("stf%d" % i, [128, 4]) for i in range(2)]; Rstf = [Reg(), Reg()]
        Rout = Reg()
        def fin_tile(ti):
            s_ = ti % 2
            rs = slice(ti * 128, (ti + 1) * 128)
            fw.dma("sp", lambda e: e.dma_start(out=at[s_][:], in_=acc_d[rs, :]), [Racc], [Rat[s_]])
            fw.dma("sp", lambda e: e.dma_start(out=ht[s_][:], in_=h_d[rs, :]), [Rh], [Rht[s_]])
            yield
            V(lambda e: e.tensor_tensor(out=at[s_][:], in0=at[s_][:], in1=mrow[:, 3 * D:4 * D], op=ALU.mult), [Rat[s_], Rmrow], [Rat[s_]])
            yield
            V(lambda e: e.tensor_tensor(out=at[s_][:], in0=at[s_][:], in1=ht[s_][:], op=ALU.add), [Rat[s_], Rht[s_]], [Rat[s_]])
            yield
            A(lambda e: e.activation(out=jf[:], in_=at[s_][:], func=AF.Square, accum_out=stf[s_][:, 0:1]), [Rat[s_]], [Rjf, Rstf[s_]])
            yield
            A(lambda e: e.activation(out=stf[s_][:, 1:2], in_=stf[s_][:, 0:1], func=AF.Sqrt, scale=1.0 / D, bias=epst[:, 0:1]), [Rstf[s_], Rc], [Rstf[s_]])
            yield
            V(lambda e: e.reciprocal(out=stf[s_][:, 2:3], in_=stf[s_][:, 1:2]), [Rstf[s_]], [Rstf[s_]])
            yield
            V(lambda e: e.scalar_tensor_tensor(out=ot[s_][:], in0=at[s_][:], scalar=stf[s_][:, 2:3], in1=mrow[:, 5 * D:6 * D], op0=ALU.mult, op1=ALU.mult),
              [Rat[s_], Rstf[s_], Rmrow], [Rot[s_]])
            yield
            fw.dma("sp", lambda e: e.dma_start(out=out_d[rs, :], in_=ot[s_][:]), [Rot[s_]], [Rout])
        for t2 in range(0, 16, 2):
            run_rr([fin_tile(t2), fin_tile(t2 + 1)])
        if dbg:
            Rd = Reg()
            d_ = dout("dbg_acc", [2048, D])
            fw.dma("sp", lambda e: e.dma_start(out=d_, in_=acc_d[0:2048, :]), [Racc], [Rd])
        fw.finish([Rout])
        fw.pop_scope()
        return nc, list(dbg_d.keys())
        if stage <= 4:
            Ro = Reg()
            fw.dma("sp", lambda e: e.dma_start(out=out_d[0:128, 0:32], in_=fm[:].rearrange("k a n -> k (a n)")), [Rfm], [Ro])
            fw.finish([Ro])
            return nc, list(dbg_d.keys())
        if stage <= 1:
            Ro = Reg()
            fw.dma("sp", lambda e: e.dma_start(out=out_d[0:128, 0:32], in_=fm[:].rearrange("k a n -> k (a n)")), [Rfm], [Ro])
            fw.finish([Ro])
            return nc, list(dbg_d.keys())
    return nc, list(dbg_d.keys())


def make_inputs(inputs):
    f = lambda a: np.ascontiguousarray(np.asarray(a, dtype=np.float32))
    x = f(inputs["x"]); c = f(inputs["c"]); ctx = f(inputs["ctx"]); c_ctx = f(inputs["c_ctx"])
    w_mod = f(inputs["w_mod"])[0]; b_mod = f(inputs["b_mod"])[0]
    w_in = f(inputs["w_in"])[0]; conv_w = f(inputs["conv_w"])[0]
    a_log = f(inputs["a_log"])[0]; dt_bias = f(inputs["dt_bias"])[0]
    gdn_g = f(inputs["gdn_norm_g"])[0]; gm_g = f(inputs["gm_norm_g"])[0]
    gm_ws = f(inputs["gm_ws"])[0]; gm_bs = f(inputs["gm_bs"])[0]
    w_out = f(inputs["w_out"])[0]
    w_router = f(inputs["w_router"])[0]; b_router = f(inputs["b_router"])[0]
    w_gate = f(inputs["w_gate"])[0]; w_up = f(inputs["w_up"])[0]; w_down = f(inputs["w_down"])[0]
    rcst = np.concatenate([(np.arange(16)[None, :] * 128 + np.arange(128)[:, None]).astype(np.float32),
                           np.tile(np.arange(SLOTS, dtype=np.float32)[None, :], (128, 1))], axis=1)
    nrm = np.concatenate([f(inputs["norm1_g"])[0], f(inputs["norm2_g"])[0], f(inputs["final_norm_g"])])[None, :]
    r = np.arange(128)
    cst = np.concatenate([
        np.eye(128), (r[None, :] >= r[:, None]), (r[None, :] <= r[:, None]), (r[None, :] > r[:, None]),
        (r[None, :] < r[:, None]), np.ones((128, 128))], axis=1).astype(np.float32)
    maps = []
    for core in range(8):
        b, h = core // 4, core % 4
        cols = np.concatenate([
            np.arange(h * 128, (h + 1) * 128),
            512 + np.arange(h * 128, (h + 1) * 128),
            1024 + np.arange(h * 128, (h + 1) * 128),
            2064 + np.arange(h * 128, (h + 1) * 128),
            1552 + np.arange(h * 128, (h + 1) * 128),
            2576 + np.arange(h * 128, (h + 1) * 128),
            np.array([1536 + h, 1540 + h, 1544 + h, 1548 + h]),
        ])
        qkv_cols = np.concatenate([np.arange(h * 128, (h + 1) * 128), 512 + np.arange(h * 128, (h + 1) * 128),
                                   1024 + np.arange(h * 128, (h + 1) * 128)])
        small = np.concatenate([
            conv_w[:, qkv_cols].reshape(-1), a_log[:, h], dt_bias[:, h], gdn_g,
            gm_g[h * 128:(h + 1) * 128], gm_bs[h]])[None, :]
        perm = np.concatenate([np.concatenate([np.arange(j * 128, (j + 1) * 128), 512 + np.arange(j * 128, (j + 1) * 128)])
                               for j in range(4)])
        maps.append({
            "x": x[b], "ctx": ctx[b],
            "crow": np.concatenate([c[b], c_ctx])[None, :].copy(),
            "w_mod": w_mod, "b_mod": b_mod[None, :].copy(), "nrm": nrm.copy(),
            "w_in": np.ascontiguousarray(w_in[:, cols]),
            "small": np.ascontiguousarray(small.astype(np.float32)),
            "gm_ws": np.ascontiguousarray(gm_ws[h]),
            "w_out": np.ascontiguousarray(w_out[perm, :]),
            "cst": cst,
            "x_own": np.ascontiguousarray(x[b, h * 2048:(h + 1) * 2048]),
            "yidx": (h * 1024 + np.arange(8)[None, :] * 128 + np.arange(128)[:, None]).astype(np.int32),
            "w_router": w_router, "b_router": b_router[None, :].copy(),
            "w_gate": w_gate, "w_up": w_up, "w_down": w_down,
            "rcst": rcst,
        })
    return maps


def kernel(**inputs):
    maps = make_inputs(inputs)
    nc, _ = build_program(stage=99)
    res = run_bass_kernel_spmd(nc, maps, core_ids=list(range(8)))
    out = np.zeros((2, T, D), np.float32)
    for core in range(8):
        b, h = core // 4, core % 4
        out[b, h * 2048:(h + 1) * 2048] = res.results[core]["out"]
    return out
```

```python
from contextlib import ExitStack
import numpy as np
import concourse.bass as bass
import concourse.mybir as mybir
from concourse.bass_utils import run_bass_kernel_spmd

F32 = mybir.dt.float32
BF16 = mybir.dt.bfloat16
I32 = mybir.dt.int32
ALU = mybir.AluOpType
AF = mybir.ActivationFunctionType
AX = mybir.AxisListType

D = 1024
T = 8192
CTX = 256
NT = 66
NE = 16
CAP = 1024
SLOTS = 320
EPS = 1e-6


class Reg:
    __slots__ = ("w", "rs")

    def __init__(self):
        self.w = None
        self.rs = {}


class FW:
    NDMA = 48

    def __init__(self, nc, stack):
        self.nc = nc
        self.stack = stack
        self.engs = {"pe": nc.tensor, "dve": nc.vector, "act": nc.scalar,
                     "pool": nc.gpsimd, "sp": nc.sync}
        self.semh = {}
        self.cnt = {}
        self.known = {e: {} for e in self.engs}
        for e in self.engs:
            self.semh[e] = stack.enter_context(nc.semaphore("p_" + e))
            self.cnt[e] = 0
        self.dma_cnt = []
        for i in range(self.NDMA):
            self.semh[("d", i)] = stack.enter_context(nc.semaphore("d%d" % i))
            self.dma_cnt.append(0)
        self.dma_rr = 0
        self.scopes = []

    def sb(self, name, shape, dt=F32):
        stk = self.scopes[-1] if self.scopes else self.stack
        return stk.enter_context(self.nc.sbuf_tensor("s_" + name, list(shape), dt))

    def push_scope(self):
        self.scopes.append(ExitStack())

    def pop_scope(self):
        self.barrier()
        self.scopes.pop().close()

    def barrier(self):
        for e in self.engs:
            for e2 in self.engs:
                if e2 != e and self.cnt[e2] > 0:
                    self.wait(e, (e2, self.cnt[e2]))
            for i in range(self.NDMA):
                if self.dma_cnt[i] > 0:
                    self.wait(e, (("d", i), 16 * self.dma_cnt[i]))

    def ps(self, name, shape, dt=F32):
        return self.stack.enter_context(self.nc.psum_tensor("ps_" + name, list(shape), dt))

    def wait(self, eng, tok):
        key, val = tok
        if key == eng and eng == "pe":
            return
        if self.known[eng].get(key, 0) >= val:
            return
        self.engs[eng].wait_ge(self.semh[key], val)
        self.known[eng][key] = val

    def _deps(self, eng, reads, writes):
        deps = {}
        for r in reads:
            if r.w is not None:
                k, v = r.w
                if deps.get(k, 0) < v:
                    deps[k] = v
        for w in writes:
            if w.w is not None:
                k, v = w.w
                if deps.get(k, 0) < v:
                    deps[k] = v
            for k, v in w.rs.items():
                if deps.get(k, 0) < v:
                    deps[k] = v
        for k, v in deps.items():
            self.wait(eng, (k, v))

    def _commit(self, tok, reads, writes):
        k, v = tok
        for r in reads:
            if r.rs.get(k, 0) < v:
                r.rs[k] = v
        for w in writes:
            w.w = tok
            w.rs = {}

    def op(self, eng, fn, reads=(), writes=()):
        self._deps(eng, reads, writes)
        ins = fn(self.engs[eng])
        self.cnt[eng] += 1
        ins.then_inc(self.semh[eng], 1)
        tok = (eng, self.cnt[eng])
        self._commit(tok, reads, writes)
        return tok

    def dma(self, eng, fn, reads=(), writes=()):
        i = self.dma_rr
        self.dma_rr = (self.dma_rr + 1) % self.NDMA
        key = ("d", i)
        if self.dma_cnt[i] > 0:
            self.wait(eng, (key, 16 * self.dma_cnt[i]))
        self._deps(eng, reads, writes)
        ins = fn(self.engs[eng])
        self.dma_cnt[i] += 1
        ins.then_inc(self.semh[key], 16)
        tok = (key, 16 * self.dma_cnt[i])
        self._commit(tok, reads, writes)
        return tok

    def cc(self, fn, reads=(), writes=()):
        if "cc" not in self.semh:
            self.semh["cc"] = self.stack.enter_context(self.nc.semaphore("cc_sem"))
            self.cnt["cc"] = 0
        if self.cnt["cc"] > 0:
            self.wait("pool", ("cc", self.cnt["cc"]))
        self._deps("pool", reads, writes)
        ins = fn(self.engs["pool"])
        self.cnt["cc"] += 1
        ins.then_inc(self.semh["cc"])
        tok = ("cc", self.cnt["cc"])
        self._commit(tok, reads, writes)
        return tok

    def finish(self, regs=()):
        for r in regs:
            if r.w is not None:
                self.wait("sp", r.w)
        for i in range(self.NDMA):
            if self.dma_cnt[i] > 0:
                self.wait("sp", (("d", i), 16 * self.dma_cnt[i]))
        for e2 in self.engs:
            if e2 != "sp" and self.cnt[e2] > 0:
                self.wait("sp", (e2, self.cnt[e2]))
        if self.cnt.get("cc", 0) > 0:
            self.wait("sp", ("cc", self.cnt["cc"]))


def build_program(stage=99, dbg=False):
    nc = bass.Bass("TRN2", target_bir_lowering=False)

    def din(name, shape, dt=F32):
        return nc.dram_tensor(name, list(shape), dt, kind="ExternalInput").ap()

    x_d = din("x", [T, D])
    ctx_d = din("ctx", [CTX, D])
    crow_d = din("crow", [1, 2 * D])
    wmod_d = din("w_mod", [D, 6 * D])
    bmod_d = din("b_mod", [1, 6 * D])
    nrm_d = din("nrm", [1, 3 * D])
    win_d = din("w_in", [D, 772])
    small_d = din("small", [1, 1152 + 4 + 128 + 128 + 128])
    gmws_d = din("gm_ws", [128, 128])
    wout_d = din("w_out", [D, D])
    cst_d = din("cst", [128, 6 * 128])
    xown_d = din("x_own", [2048, D])
    yidx_d = din("yidx", [128, 8], I32)
    wr_d = din("w_router", [D, NE])
    br_d = din("b_router", [1, NE])
    wg_d = din("w_gate", [NE, D, D])
    wu_d = din("w_up", [NE, D, D])
    wd_d = din("w_down", [NE, D, D])
    rc_d = din("rcst", [128, 16 + SLOTS])
    out_d = nc.dram_tensor("out", [2048, D], F32, kind="ExternalOutput").ap()
    dbg_d = {}

    def dout(name, shape, dt=F32):
        dbg_d[name] = nc.dram_tensor(name, list(shape), dt, kind="ExternalOutput").ap()
        return dbg_d[name]

    PRE_W = 8452
    pre_d = nc.dram_tensor("pre_scr", [3, 128, PRE_W], F32).ap()
    modrow_d = nc.dram_tensor("modrow_scr", [128, 6 * D], F32).ap()

    with ExitStack() as st:
        fw = FW(nc, st)
        sb, ps = fw.sb, fw.ps
        P = lambda fn, r=(), w=(): fw.op("pe", fn, r, w)
        V = lambda fn, r=(), w=(): fw.op("dve", fn, r, w)
        A = lambda fn, r=(), w=(): fw.op("act", fn, r, w)
        G = lambda fn, r=(), w=(): fw.op("pool", fn, r, w)

        def run_rr(gens):
            gens = list(gens)
            while gens:
                for g_ in list(gens):
                    try:
                        next(g_)
                    except StopIteration:
                        gens.remove(g_)

        cst = sb("cst", [128, 6 * 128]); Rc = Reg()
        fw.dma("sp", lambda e: e.dma_start(out=cst[:], in_=cst_d), writes=[Rc])
        ident = cst[:, 0:128]
        U_in = cst[:, 128:256]
        L_in = cst[:, 256:384]
        U_st = cst[:, 384:512]
        L_st = cst[:, 512:640]
        ones = cst[:, 640:768]
        identb = sb("identb", [128, 128], BF16)
        V(lambda e: e.tensor_copy(out=identb[:], in_=ident), [Rc], [Rc])
        epst = sb("epst", [128, 1])
        V(lambda e: e.memset(epst[:], EPS), [], [Rc])
        onec = sb("onec", [128, 1])
        V(lambda e: e.memset(onec[:], 1.0), [], [Rc])

        bank = [ps("bank%d" % i, [128, 512]) for i in range(8)]
        Rb = [Reg() for _ in range(8)]

        fm = sb("fm", [128, 16, 2]); Rfm = Reg()
        g1 = sb("g1", [128, 8]); Rg1 = Reg()
        s1 = sb("s1", [128, 8, 2]); Rs1 = Reg()
        cw = sb("cw", [128, 16]); Rcw = Reg()
        srow = sb("srow", [128, 388]); Rsrow = Reg()
        nexpA = sb("nexpA", [128, 2]); RnA = Reg()
        fw.push_scope()
        uT = sb("uT", [128, T], BF16); RuT = Reg()
        zs = sb("zs", [128, 64, 128], BF16); Rzs = Reg()
        vgn = sb("vgn", [128, 64, 128], BF16); Rvgn = Reg()
        ab = sb("ab", [128, NT, 4]); Rab = Reg()
        fw.push_scope()
        wsb = sb("wsb", [128, 8, 772], BF16); Rw = Reg()
        fw.push_scope()
        rows = sb("rows", [128, 4 * D]); Rrows = Reg()
        g2F = sb("g2F", [128, 2 * D]); Rg2F = Reg()
        crow = sb("crow", [1, 2 * D]); Rcrow = Reg()
        fw.dma("sp", lambda e: e.dma_start(out=crow[:], in_=crow_d), writes=[Rcrow])
        bmod = sb("bmod", [1, 6 * D]); Rbm = Reg()
        fw.dma("sp", lambda e: e.dma_start(out=bmod[:], in_=bmod_d), writes=[Rbm])
        nrm = sb("nrm", [1, 3 * D]); Rnrm = Reg()
        fw.dma("sp", lambda e: e.dma_start(out=nrm[:], in_=nrm_d), writes=[Rnrm])
        small = sb("small", [1, 1540]); Rsm = Reg()
        fw.dma("sp", lambda e: e.dma_start(out=small[:], in_=small_d), writes=[Rsm])
        fw.dma("pool", lambda e: e.dma_start(out=wsb[:], in_=win_d.rearrange("(j k) n -> k j n", k=128)), writes=[Rw])

        A(lambda e: e.activation(out=crow[:], in_=crow[:], func=AF.Silu), [Rcrow], [Rcrow])
        scv = sb("scv", [128, 8, 2]); Rscv = Reg()
        for j in range(8):
            for n in range(2):
                P(lambda e: e.matmul(bank[0][:, j * 2 + n:j * 2 + n + 1], lhsT=crow[0:1, n * D + j * 128:n * D + (j + 1) * 128],
                                     rhs=ones[0:1, 0:1], start=True, stop=True), [Rcrow, Rc], [Rb[0]])
        V(lambda e: e.tensor_copy(out=scv[:].rearrange("k j n -> k (j n)"), in_=bank[0][:, 0:16]), [Rb[0]], [Rscv])
        scb = sb("scb", [128, 8, 128]); Rscb = Reg()
        for j in range(8):
            V(lambda e: e.tensor_copy(out=scb[:, j, :], in_=scv[:, j, 0:1].to_broadcast([128, 128])), [Rscv], [Rscb])

        wm = [sb("wm%d" % i, [128, 8, 512]) for i in range(2)]
        Rwm = [Reg(), Reg()]
        wmod_v = wmod_d.rearrange("(j k) n -> k j n", k=128)
        for pc in range(12):
            s = pc % 2
            fw.dma("sp", lambda e: e.dma_start(out=wm[s][:], in_=wmod_v[:, :, pc * 512:(pc + 1) * 512]), writes=[Rwm[s]])
            bk = 1 + (pc % 2)
            if pc < 4:
                for m in range(4):
                    for j in range(8):
                        P(lambda e: e.matmul(bank[bk][:, m * 2:m * 2 + 2], lhsT=wm[s][:, j, m * 128:(m + 1) * 128], rhs=scv[:, j, :],
                                             start=(j == 0), stop=False), [Rwm[s], Rscv], [Rb[bk]])
                    c0 = pc * 512 + m * 128
                    P(lambda e: e.matmul(bank[bk][:, m * 2:m * 2 + 2], lhsT=bmod[0:1, c0:c0 + 128], rhs=ones[0:1, 0:2],
                                         start=False, stop=True), [Rbm, Rc], [Rb[bk]])
                V(lambda e: e.tensor_copy(out=fm[:, pc * 4:(pc + 1) * 4, :].rearrange("k a n -> k (a n)"), in_=bank[bk][:, 0:8]),
                  [Rb[bk]], [Rfm])
            else:
                for j in range(8):
                    P(lambda e: e.matmul(bank[bk][:, :], lhsT=scb[:, j, :], rhs=wm[s][:, j, :], start=(j == 0), stop=False),
                      [Rwm[s], Rscb], [Rb[bk]])
                c0 = pc * 512
                P(lambda e: e.matmul(bank[bk][:, :], lhsT=ones[0:1, :], rhs=bmod[0:1, c0:c0 + 512], start=False, stop=True),
                  [Rbm, Rc], [Rb[bk]])
                A(lambda e: e.activation(out=rows[:, (pc - 4) * 512:(pc - 3) * 512], in_=bank[bk][:, :], func=AF.Copy), [Rb[bk]], [Rrows])
        for j in range(8):
            P(lambda e: e.matmul(bank[3][:, j:j + 1], lhsT=nrm[0:1, j * 128:(j + 1) * 128], rhs=ones[0:1, 0:1], start=True, stop=True),
              [Rnrm, Rc], [Rb[3]])
        V(lambda e: e.tensor_copy(out=g1[:], in_=bank[3][:, 0:8]), [Rb[3]], [Rg1])
        for n in range(2):
            V(lambda e: e.scalar_tensor_tensor(out=s1[:, :, n], in0=fm[:, 8:16, n], scalar=1.0, in1=g1[:], op0=ALU.add, op1=ALU.mult),
              [Rfm, Rg1], [Rs1])
        for q in range(4):
            P(lambda e: e.matmul(bank[4][:, :], lhsT=ones[0:1, :], rhs=nrm[0:1, D + q * 512:D + (q + 1) * 512], start=True, stop=True),
              [Rnrm, Rc], [Rb[4]])
            A(lambda e: e.activation(out=g2F[:, q * 512:(q + 1) * 512], in_=bank[4][:, :], func=AF.Copy), [Rb[4]], [Rg2F])
        V(lambda e: e.scalar_tensor_tensor(out=rows[:, 2 * D:3 * D], in0=rows[:, 2 * D:3 * D], scalar=1.0, in1=g2F[:, 0:D],
                                           op0=ALU.add, op1=ALU.mult), [Rrows, Rg2F], [Rrows])
        for sidx in range(3):
            for tap in range(3):
                c0 = tap * 384 + sidx * 128
                P(lambda e: e.matmul(bank[5][:, sidx * 3 + tap:sidx * 3 + tap + 1], lhsT=small[0:1, c0:c0 + 128], rhs=ones[0:1, 0:1],
                                     start=True, stop=True), [Rsm, Rc], [Rb[5]])
        P(lambda e: e.matmul(bank[5][:, 9:10], lhsT=small[0:1, 1156:1284], rhs=ones[0:1, 0:1], start=True, stop=True), [Rsm, Rc], [Rb[5]])
        V(lambda e: e.tensor_copy(out=cw[:, 0:10], in_=bank[5][:, 0:10]), [Rb[5]], [Rcw])
        P(lambda e: e.matmul(bank[6][:, 0:388], lhsT=ones[0:1, :], rhs=small[0:1, 1152:1540], start=True, stop=True), [Rsm, Rc], [Rb[6]])
        V(lambda e: e.tensor_copy(out=srow[:], in_=bank[6][:, 0:388]), [Rb[6]], [Rsrow])
        A(lambda e: e.activation(out=nexpA[:], in_=srow[:, 0:2], func=AF.Exp), [Rsrow], [RnA])
        V(lambda e: e.tensor_scalar(out=nexpA[:], in0=nexpA[:], scalar1=-1.0, scalar2=None, op0=ALU.mult), [RnA], [RnA])
        Rmodrow = Reg()
        fw.dma("sp", lambda e: e.dma_start(out=modrow_d[:, 0:4 * D], in_=rows[:]), [Rrows], [Rmodrow])
        fw.dma("sp", lambda e: e.dma_start(out=modrow_d[:, 4 * D:6 * D], in_=g2F[:]), [Rg2F], [Rmodrow])

        if dbg:
            d = dout("dbg_fm", [128, 32]); Rd = Reg()
            fw.dma("sp", lambda e: e.dma_start(out=d, in_=fm[:].rearrange("k a n -> k (a n)")), [Rfm], [Rd])
            d2 = dout("dbg_rows", [128, 4 * D])
            fw.dma("sp", lambda e: e.dma_start(out=d2, in_=rows[:]), [Rrows], [Rd])
            d3 = dout("dbg_s1", [128, 16])
            fw.dma("sp", lambda e: e.dma_start(out=d3, in_=s1[:].rearrange("k a n -> k (a n)")), [Rs1], [Rd])

        fw.pop_scope()
        fw.push_scope()
        xt = [sb("xt%d" % i, [128, D]) for i in range(2)]; Rxt = [Reg(), Reg()]
        junk = sb("junk", [128, D]); Rjunk = Reg()
        stat = [sb("stat%d" % i, [128, 4]) for i in range(2)]; Rstat = [Reg(), Reg()]
        xn = [sb("xn%d" % i, [128, D], BF16) for i in range(2)]; Rxn = [Reg(), Reg()]
        aT = [sb("aT%d" % i, [128, 8, 512], BF16) for i in range(2)]; RaTt = [[Reg() for _ in range(4)] for _ in range(2)]
        prew = [sb("prew%d" % i, [128, 3, 512]) for i in range(2)]; Rprew = [Reg(), Reg()]
        tmg4 = [sb("tmg4_%d" % i, [128, 256]) for i in range(4)]; Rtmg4 = [Reg() for _ in range(4)]
        st4 = sb("st4", [128, 4, 4]); Rst4 = [Reg() for _ in range(4)]
        zero = sb("zero", [128, 4]); Rz = Reg()
        V(lambda e: e.memset(zero[:], 0.0), [], [Rz])
        Rpre = Reg()
        for sidx in range(3):
            for col in (0, 257, 258, 8451):
                fw.dma("sp", lambda e: e.dma_start(out=pre_d[sidx, :, col:col + 1], in_=zero[:, 0:1], allow_slow_non_contiguous=True), [Rz], [Rpre])

        blocks = [(ctx_d, 0, 2, 1, 0, 1)]
        for bi in range(16):
            blocks.append((x_d, bi * 512, 4, 0, 2 + bi * 4, 259 + bi * 512))
        tcount = 0
        for bidx, (src, row0, nb, mn, t0, pc0) in enumerate(blocks):
            bs = bidx % 2
            def a1_tile(ti, s, bkT):
                r0 = row0 + ti * 128
                fw.dma("sp", lambda e: e.dma_start(out=xt[s][:], in_=src[r0:r0 + 128, :]), writes=[Rxt[s]])
                yield
                A(lambda e: e.activation(out=junk[:], in_=xt[s][:], func=AF.Square, accum_out=stat[s][:, 0:1]), [Rxt[s]], [Rjunk, Rstat[s]])
                yield
                A(lambda e: e.activation(out=stat[s][:, 1:2], in_=stat[s][:, 0:1], func=AF.Sqrt, scale=1.0 / D, bias=epst[:, 0:1]),
                  [Rstat[s], Rc], [Rstat[s]])
                yield
                V(lambda e: e.reciprocal(out=stat[s][:, 2:3], in_=stat[s][:, 1:2]), [Rstat[s]], [Rstat[s]])
                yield
                A(lambda e: e.activation(out=xn[s][:], in_=xt[s][:], func=AF.Copy, scale=stat[s][:, 2:3]), [Rxt[s], Rstat[s]], [Rxn[s]])
                yield
                pT = bank[bkT][:, :].bitcast(BF16)
                for j in range(8):
                    P(lambda e: e.transpose(out=pT[:, j * 128:(j + 1) * 128], in_=xn[s][:, j * 128:(j + 1) * 128], identity=identb[:]),
                      [Rxn[s], Rc], [Rb[bkT]])
                yield
                for j in range(8):
                    if j % 4 != 3:
                        V(lambda e: e.tensor_scalar(out=aT[bs][:, j, ti * 128:(ti + 1) * 128], in0=pT[:, j * 128:(j + 1) * 128],
                                                    scalar1=s1[:, j, mn:mn + 1], scalar2=fm[:, j, mn:mn + 1], op0=ALU.mult, op1=ALU.add),
                          [Rb[bkT], Rs1, Rfm], [RaTt[bs][ti]])
                    else:
                        A(lambda e: e.activation(out=aT[bs][:, j, ti * 128:(ti + 1) * 128], in_=pT[:, j * 128:(j + 1) * 128],
                                                 func=AF.Identity, scale=s1[:, j, mn:mn + 1], bias=fm[:, j, mn:mn + 1]),
                          [Rb[bkT], Rs1, Rfm], [RaTt[bs][ti]])
            for t2 in range(0, nb, 2):
                run_rr([a1_tile(t2, 0, 7), a1_tile(t2 + 1, 1, 6)])
            N = nb * 128
            for gi in range(4):
                bk = gi % 2
                for j in range(8):
                    P(lambda e: e.matmul(bank[bk][:, 0:N], lhsT=wsb[:, j, gi * 128:(gi + 1) * 128], rhs=aT[bs][:, j, 0:N],
                                         start=(j == 0), stop=(j == 7)), [Rw] + RaTt[bs][0:nb], [Rb[bk]])
                if gi < 3:
                    if gi % 2 == 0:
                        V(lambda e: e.tensor_copy(out=prew[bs][:, gi, 0:N], in_=bank[bk][:, 0:N]), [Rb[bk]], [Rprew[bs]])
                    else:
                        A(lambda e: e.activation(out=prew[bs][:, gi, 0:N], in_=bank[bk][:, 0:N], func=AF.Copy), [Rb[bk]], [Rprew[bs]])
                elif mn == 0:
                    A(lambda e: e.activation(out=uT[:, row0:row0 + N], in_=bank[bk][:, 0:N], func=AF.Gelu), [Rb[bk]], [RuT])
            fw.dma("sp", lambda e: e.dma_start(out=pre_d[:, :, pc0:pc0 + N].rearrange("s p n -> p s n"), in_=prew[bs][:, :, 0:N]),
                   [Rprew[bs]], [Rpre])
            def tm_tile(ti):
                tix = t0 + ti
                bk = 2 + ti
                for j in range(8):
                    P(lambda e: e.matmul(bank[bk][:, 0:260], lhsT=aT[bs][:, j, ti * 128:(ti + 1) * 128], rhs=wsb[:, j, 512:772],
                                         start=(j == 0), stop=(j == 7)), [Rw] + RaTt[bs][0:nb], [Rb[bk]])
                yield
                V(lambda e: e.tensor_copy(out=ab[:, tix, :], in_=bank[bk][:, 256:260]), [], [Rab, Rb[bk]])
                if mn != 0:
                    return
                lt = tix - 2
                yield
                A(lambda e: e.activation(out=zs[:, lt, :], in_=bank[bk][:, 0:128], func=AF.Silu), [], [Rzs, Rb[bk]])
                yield
                A(lambda e: e.activation(out=tmg4[ti][:, 0:128], in_=bank[bk][:, 128:256], func=AF.Gelu), [], [Rtmg4[ti], Rb[bk]])
                yield
                V(lambda e: e.tensor_tensor(out=tmg4[ti][:, 128:256], in0=tmg4[ti][:, 0:128], in1=tmg4[ti][:, 0:128], op=ALU.mult), [Rtmg4[ti]], [Rtmg4[ti]])
                yield
                V(lambda e: e.tensor_reduce(out=st4[:, ti, 0:1], in_=tmg4[ti][:, 128:256], axis=AX.X, op=ALU.add), [Rtmg4[ti]], [Rst4[ti]])
                yield
                A(lambda e: e.activation(out=st4[:, ti, 1:2], in_=st4[:, ti, 0:1], func=AF.Sqrt, scale=1.0 / 128, bias=epst[:, 0:1]),
                  [Rst4[ti], Rc], [Rst4[ti]])
                yield
                V(lambda e: e.reciprocal(out=st4[:, ti, 2:3], in_=st4[:, ti, 1:2]), [Rst4[ti]], [Rst4[ti]])
                yield
                V(lambda e: e.scalar_tensor_tensor(out=vgn[:, lt, :], in0=tmg4[ti][:, 0:128], scalar=st4[:, ti, 2:3], in1=srow[:, 132:260],
                                                   op0=ALU.mult, op1=ALU.mult), [Rtmg4[ti], Rst4[ti], Rsrow], [Rvgn])
            run_rr([tm_tile(ti) for ti in range(nb)])
        if dbg:
            Rd = Reg()
            d = dout("dbg_ab", [128, NT * 4])
            fw.dma("sp", lambda e: e.dma_start(out=d, in_=ab[:].rearrange("p t c -> p (t c)")), [Rab], [Rd])
            d = dout("dbg_uT", [128, T], BF16)
            fw.dma("sp", lambda e: e.dma_start(out=d, in_=uT[:]), [RuT], [Rd])
            d = dout("dbg_zs", [128, 64 * 128], BF16)
            fw.dma("sp", lambda e: e.dma_start(out=d, in_=zs[:].rearrange("p t c -> p (t c)")), [Rzs], [Rd])
            d = dout("dbg_vgn", [128, 64 * 128], BF16)
            fw.dma("sp", lambda e: e.dma_start(out=d, in_=vgn[:].rearrange("p t c -> p (t c)")), [Rvgn], [Rd])
            d = dout("dbg_pre", [3, 128, PRE_W])
            fw.dma("sp", lambda e: e.dma_start(out=d, in_=pre_d), [Rpre], [Rd])
            fw.finish([Rd])
        fw.pop_scope()
        fw.pop_scope()
        if stage >= 2:
            oacc = sb("oacc", [128, 64, 128]); Roacc = [Reg() for _ in range(64)]
            fw.push_scope()
            qT = sb("qT", [128, NT * 128], BF16); RqT = Reg()
            kT = sb("kT", [128, NT * 128], BF16); RkT = Reg()
            ktok = sb("ktok", [128, NT, 128], BF16); Rktok = Reg()
            vtok = sb("vtok", [128, NT, 128], BF16); Rvtok = Reg()
            fw.push_scope()
            pre3 = [sb("pre3_%d" % i, [128, 3, 514]) for i in range(2)]; Rpre3 = [Reg(), Reg()]
            cacc = [[sb("cacc%d_%d" % (p_, i), [128, 512]) for i in range(3)] for p_ in range(2)]; Rcacc = [[Reg() for _ in range(3)] for _ in range(2)]
            sl = [[sb("sl%d_%d" % (p_, i), [128, 512]) for i in range(2)] for p_ in range(2)]; Rsl = [[Reg() for _ in range(2)] for _ in range(2)]
            sq = [[sb("sq%d_%d" % (p_, i), [128, 512]) for i in range(2)] for p_ in range(2)]; Rsq = [[Reg() for _ in range(2)] for _ in range(2)]
            rn = [[sb("rn%d_%d" % (p_, i), [128, 512]) for i in range(2)] for p_ in range(2)]; Rrn = [[Reg() for _ in range(2)] for _ in range(2)]
            vbf = [sb("vbf%d" % p_, [128, 512], BF16) for p_ in range(2)]; Rvbf = [Reg(), Reg()]
            blocks2 = [(1, 256, 0)] + [(259 + bi * 512, 512, 2 + bi * 4) for bi in range(16)]

            def a2_stream(bidx, sidx):
                c0, N, t0 = blocks2[bidx]
                p_ = bidx % 2
                ca, Rca = cacc[p_][sidx], Rcacc[p_][sidx]
                if sidx == 0:
                    fw.dma("sp", lambda e: e.dma_start(out=pre3[p_][:, :, 0:N + 2], in_=pre_d[:, :, c0 - 1:c0 + N + 1].rearrange("s p n -> p s n")),
                           [Rpre], [Rpre3[p_]])
                yield
                V(lambda e: e.tensor_scalar(out=ca[:, 0:N], in0=pre3[p_][:, sidx, 0:N], scalar1=cw[:, sidx * 3:sidx * 3 + 1],
                                            scalar2=None, op0=ALU.mult), [Rpre3[p_], Rcw], [Rca])
                for tap in (1, 2):
                    yield
                    V(lambda e: e.scalar_tensor_tensor(out=ca[:, 0:N], in0=pre3[p_][:, sidx, tap:tap + N],
                                                       scalar=cw[:, sidx * 3 + tap:sidx * 3 + tap + 1], in1=ca[:, 0:N],
                                                       op0=ALU.mult, op1=ALU.add), [Rpre3[p_], Rcw, Rca], [Rca])
                yield
                if sidx == 2:
                    bkv = 7 if p_ == 0 else 3
                    A(lambda e: e.activation(out=vbf[p_][:, 0:N], in_=ca[:, 0:N], func=AF.Silu), [Rca], [Rvbf[p_]])
                    yield
                    pTb = bank[bkv][:, :].bitcast(BF16)
                    for ti in range(N // 128):
                        P(lambda e: e.transpose(out=pTb[:, ti * 128:(ti + 1) * 128], in_=vbf[p_][:, ti * 128:(ti + 1) * 128], identity=identb[:]), [Rvbf[p_], Rc], [Rb[bkv]])
                    yield
                    V(lambda e: e.tensor_copy(out=vtok[:, t0:t0 + N // 128, :], in_=pTb[:, 0:N].rearrange("p (t c) -> p t c", c=128)), [Rb[bkv]], [Rvtok])
                    return
                bk = (4 + sidx) if p_ == 0 else sidx
                A(lambda e: e.activation(out=sl[p_][sidx][:, 0:N], in_=ca[:, 0:N], func=AF.Silu), [Rca], [Rsl[p_][sidx]])
                yield
                A(lambda e: e.activation(out=sq[p_][sidx][:, 0:N], in_=sl[p_][sidx][:, 0:N], func=AF.Square), [Rsl[p_][sidx]], [Rsq[p_][sidx]])
                yield
                P(lambda e: e.matmul(bank[bk][:, 0:N], lhsT=ones, rhs=sq[p_][sidx][:, 0:N], start=True, stop=True), [Rsq[p_][sidx], Rc], [Rb[bk]])
                yield
                A(lambda e: e.activation(out=rn[p_][sidx][:, 0:N], in_=bank[bk][:, 0:N], func=AF.Sqrt, bias=epst[:, 0:1]), [Rb[bk], Rc], [Rrn[p_][sidx]])
                yield
                V(lambda e: e.reciprocal(out=rn[p_][sidx][:, 0:N], in_=rn[p_][sidx][:, 0:N]), [Rrn[p_][sidx]], [Rrn[p_][sidx]])
                yield
                dst, Rdst = (qT, RqT) if sidx == 0 else (kT, RkT)
                scl = 128.0 ** -0.5 if sidx == 0 else 1.0
                V(lambda e: e.scalar_tensor_tensor(out=dst[:, t0 * 128:t0 * 128 + N], in0=sl[p_][sidx][:, 0:N], scalar=scl, in1=rn[p_][sidx][:, 0:N],
                                                   op0=ALU.mult, op1=ALU.mult), [Rsl[p_][sidx], Rrn[p_][sidx]], [Rdst])
                if sidx == 1:
                    yield
                    bkk = 6 if p_ == 0 else 2
                    pTb = bank[bkk][:, :].bitcast(BF16)
                    for ti in range(N // 128):
                        P(lambda e: e.transpose(out=pTb[:, ti * 128:(ti + 1) * 128], in_=kT[:, (t0 + ti) * 128:(t0 + ti + 1) * 128], identity=identb[:]),
                          [RkT, Rc], [Rb[bkk]])
                    yield
                    A(lambda e: e.activation(out=ktok[:, t0:t0 + N // 128, :], in_=pTb[:, 0:N].rearrange("p (t c) -> p t c", c=128), func=AF.Copy), [Rb[bkk]], [Rktok])

            run_rr([a2_stream(0, 0), a2_stream(0, 1), a2_stream(0, 2)])
            for b2 in range(1, 17, 2):
                run_rr([a2_stream(bb, ss) for bb in (b2, b2 + 1) for ss in range(3)])
            fw.pop_scope()
            import os as _os
            if _os.environ.get("KSTOP") == "a2":
                Ro = Reg()
                fw.dma("sp", lambda e: e.dma_start(out=out_d[0:128, 0:32], in_=fm[:].rearrange("k a n -> k (a n)")), [Rfm, RqT, RkT, Rktok, Rvtok], [Ro])
                fw.finish([Ro])
                return nc, list(dbg_d.keys())
            gsc = {}
            Rgs = Reg()
            abT = sb("abT", [128, 4, NT]); RabT = Reg()
            for c_ in range(4):
                V(lambda e: e.tensor_copy(out=abT[:, c_, :], in_=ab[:, :, c_]), [Rab], [RabT])
            for d in range(2):
                for nm in ("g", "beta", "gc", "neggc", "eg", "ekg", "egl", "bkg", "negbeta", "tmp"):
                    gsc[(nm, d)] = sb("gs_%s%d" % (nm, d), [128, NT])
                gs = lambda nm: gsc[(nm, d)]
                A(lambda e: e.activation(out=gs("tmp")[:], in_=abT[:, d, :], func=AF.Exp, bias=srow[:, 2 + d:3 + d]), [RabT, Rsrow], [Rgs])
                A(lambda e: e.activation(out=gs("tmp")[:], in_=gs("tmp")[:], func=AF.Ln, bias=onec[:, 0:1]), [Rgs, Rc], [Rgs])
                V(lambda e: e.tensor_scalar(out=gs("g")[:], in0=gs("tmp")[:], scalar1=nexpA[:, d:d + 1], scalar2=None, op0=ALU.mult), [Rgs, RnA], [Rgs])
                A(lambda e: e.activation(out=gs("beta")[:], in_=abT[:, 2 + d, :], func=AF.Sigmoid), [RabT], [Rgs])
                tri = U_in if d == 0 else L_in
                P(lambda e: e.matmul(bank[0][:, 0:NT], lhsT=tri, rhs=gs("g")[:], start=True, stop=True), [Rgs, Rc], [Rb[0]])
                P(lambda e: e.matmul(bank[0][:, 128:128 + NT], lhsT=ones, rhs=gs("g")[:], start=True, stop=True), [Rgs, Rc], [Rb[0]])
                V(lambda e: e.tensor_copy(out=gs("gc")[:], in_=bank[0][:, 0:NT]), [Rb[0]], [Rgs])
                V(lambda e: e.tensor_scalar(out=gs("neggc")[:], in0=bank[0][:, 0:NT], scalar1=-1.0, scalar2=None, op0=ALU.mult), [Rb[0]], [Rgs])
                A(lambda e: e.activation(out=gs("eg")[:], in_=bank[0][:, 0:NT], func=AF.Exp), [Rb[0]], [Rgs])
                A(lambda e: e.activation(out=gs("egl")[:], in_=bank[0][:, 128:128 + NT], func=AF.Exp), [Rb[0]], [Rgs])
                V(lambda e: e.tensor_tensor(out=gs("tmp")[:], in0=bank[0][:, 128:128 + NT], in1=gs("gc")[:], op=ALU.subtract), [Rb[0], Rgs], [Rgs])
                A(lambda e: e.activation(out=gs("ekg")[:], in_=gs("tmp")[:], func=AF.Exp), [Rgs], [Rgs])
                V(lambda e: e.tensor_tensor(out=gs("bkg")[:], in0=gs("beta")[:], in1=gs("eg")[:], op=ALU.mult), [Rgs], [Rgs])
                V(lambda e: e.tensor_scalar(out=gs("negbeta")[:], in0=gs("beta")[:], scalar1=-1.0, scalar2=None, op0=ALU.mult), [Rgs], [Rgs])
            if dbg:
                Rd = Reg()
                d_ = dout("dbg_qT", [128, NT * 128], BF16)
                fw.dma("sp", lambda e: e.dma_start(out=d_, in_=qT[:]), [RqT], [Rd])
                d_ = dout("dbg_kT", [128, NT * 128], BF16)
                fw.dma("sp", lambda e: e.dma_start(out=d_, in_=kT[:]), [RkT], [Rd])
                d_ = dout("dbg_ktok", [128, NT * 128], BF16)
                fw.dma("sp", lambda e: e.dma_start(out=d_, in_=ktok[:].rearrange("p t c -> p (t c)")), [Rktok], [Rd])
                d_ = dout("dbg_vtok", [128, NT * 128], BF16)
                fw.dma("sp", lambda e: e.dma_start(out=d_, in_=vtok[:].rearrange("p t c -> p (t c)")), [Rvtok], [Rd])
                for nm in ("g", "beta", "gc", "egl", "ekg"):
                    for d in range(2):
                        d_ = dout("dbg_%s%d" % (nm, d), [128, NT])
                        fw.dma("sp", lambda e: e.dma_start(out=d_, in_=gsc[(nm, d)][:]), [Rgs], [Rd])
                fw.finish([Rd])
            fw.barrier()
            DTI = F32
            fw.push_scope()
            Rq = [[rb_] * 4 for rb_ in [Reg() for _ in range(8)]]
            qv = lambda b_, q_: bank[b_][:, q_ * 128:(q_ + 1) * 128]
            W = {}
            RW = {}
            PREPB = (("dgc", F32), ("dng", F32), ("m1", F32), ("m2", F32), ("E1m", F32), ("E2m", F32), ("N", DTI), ("Nt", DTI), ("N2", DTI),
                     ("Nt2", DTI), ("Qa", DTI), ("Qb", DTI), ("TT", BF16), ("kbg", BF16), ("vb", BF16))
            HOB = (("u", F32), ("wT", BF16), ("attnT", BF16), ("kg", BF16))
            SEQB = (("vnew", BF16), ("S", F32), ("Sb", BF16), ("otmp", F32), ("otmp2", F32))
            NCTX, NSLOT = 2, 3
            ALIAS = {}
            for d in range(2):
                for c_ in range(NCTX):
                    for nm, dt_ in PREPB:
                        if nm in ALIAS:
                            continue
                        W[(nm, d, c_)] = sb("w_%s%d_%d" % (nm, d, c_), [128, 128], dt_); RW[(nm, d, c_)] = Reg()
                    for nm, tgt in ALIAS.items():
                        W[(nm, d, c_)] = W[(tgt, d, c_)]; RW[(nm, d, c_)] = RW[(tgt, d, c_)]
                for sl_ in range(NSLOT):
                    for nm, dt_ in HOB:
                        W[(nm, d, "h", sl_)] = sb("h_%s%d_%d" % (nm, d, sl_), [128, 128], dt_); RW[(nm, d, "h", sl_)] = Reg()
                for nm, dt_ in SEQB:
                    W[(nm, d)] = sb("q_%s%d" % (nm, d), [128, 128], dt_); RW[(nm, d)] = Reg()
                V(lambda e: e.memset(W[("S", d)][:], 0.0), [], [RW[("S", d)]])
                V(lambda e: e.memset(W[("Sb", d)][:], 0.0), [], [RW[("Sb", d)]])
            identI = ident if DTI == F32 else identb[:]
            touched = set()
            HO = ("u", "wT", "attnT", "kg")
            bankregs = set(id(x[0]) for x in Rq)

            def excl(E):
                def f(fn, r=(), w=()):
                    return E(fn, [x for x in r if id(x) not in bankregs], list(w) + [x for x in r if id(x) in bankregs])
                return f
            V0 = V
            V, A = excl(V), excl(A)
            Rmk = Reg()
            nmT = [sb("nmT%d" % d_, [128, 128]) for d_ in range(2)]
            pmS = [sb("pmS%d" % d_, [128, 128]) for d_ in range(2)]
            for d_ in range(2):
                V0(lambda e: e.tensor_scalar(out=nmT[d_][:], in0=(U_in if d_ == 0 else L_in), scalar1=-1.0, scalar2=200.0, op0=ALU.add, op1=ALU.mult), [Rc], [Rmk])
                V0(lambda e: e.tensor_scalar(out=pmS[d_][:], in0=(L_st if d_ == 0 else U_st), scalar1=-1.0, scalar2=-200.0, op0=ALU.add, op1=ALU.mult), [Rc], [Rmk])

            def prep(n, d, c_, sl_):
                w = lambda nm: (W[(nm, d, "h", sl_)] if nm in HO else W[(nm, d, c_)])[:]
                r = lambda nm: RW[(nm, d, "h", sl_)] if nm in HO else RW[(nm, d, c_)]
                gs = lambda nm: gsc[(nm, d)][:, n:n + 1]
                bk = d * NCTX + c_
                Rk = Rq[bk][0]
                tk = slice(n * 128, (n + 1) * 128)
                P(lambda e: e.matmul(qv(bk, 0), lhsT=kT[:, tk], rhs=kT[:, tk], start=True, stop=True), [RkT], [Rk])
                P(lambda e: e.matmul(qv(bk, 1), lhsT=kT[:, tk], rhs=qT[:, tk], start=True, stop=True), [RkT, RqT], [Rk])
                V(lambda e: e.tensor_scalar(out=w("dgc"), in0=ident, scalar1=gs("gc"), scalar2=None, op0=ALU.mult), [Rc, Rgs], [r("dgc")])
                A(lambda e: e.activation(out=w("dng"), in_=ident, func=AF.Copy, scale=gs("neggc")), [Rc, Rgs], [r("dng")])
                A(lambda e: e.activation(out=w("kbg"), in_=ktok[:, n, :], func=AF.Copy, scale=gs("bkg")), [Rktok, Rgs], [r("kbg")])
                G(lambda e: e.tensor_scalar(out=w("kg"), in0=ktok[:, n, :], scalar1=gs("ekg"), scalar2=None, op0=ALU.mult), [Rktok, Rgs], [r("kg")])
                G(lambda e: e.tensor_scalar(out=w("vb"), in0=vtok[:, n, :], scalar1=gs("beta"), scalar2=None, op0=ALU.mult), [Rvtok, Rgs], [r("vb")])
                yield
                P(lambda e: e.matmul(qv(bk, 2), lhsT=ones, rhs=w("dgc"), start=True, stop=False), [Rc, r("dgc")], [Rk])
                P(lambda e: e.matmul(qv(bk, 2), lhsT=w("dng"), rhs=ones, start=False, stop=True), [Rc, r("dng")], [Rk])
                yield
                V(lambda e: e.scalar_tensor_tensor(out=w("m2"), in0=qv(bk, 2), scalar=0.0, in1=pmS[d][:], op0=ALU.max, op1=ALU.add), [Rk, Rmk], [r("m2")])
                V(lambda e: e.scalar_tensor_tensor(out=w("m1"), in0=qv(bk, 2), scalar=0.0, in1=nmT[d][:], op0=ALU.min, op1=ALU.add), [Rk, Rmk], [r("m1")])
                yield
                A(lambda e: e.activation(out=w("E2m"), in_=w("m2"), func=AF.Exp, scale=-1.0), [r("m2")], [r("E2m")])
                A(lambda e: e.activation(out=w("E1m"), in_=w("m1"), func=AF.Exp), [r("m1")], [r("E1m")])
                yield
                V(lambda e: e.scalar_tensor_tensor(out=w("N"), in0=qv(bk, 0), scalar=gs("negbeta"), in1=w("E2m"), op0=ALU.mult, op1=ALU.mult),
                  [Rk, Rgs, r("E2m")], [r("N")])
                V(lambda e: e.tensor_tensor(out=w("attnT"), in0=qv(bk, 1), in1=w("E1m"), op=ALU.mult), [Rk, r("E1m")], [r("attnT")])
                yield
                P(lambda e: e.matmul(qv(bk, 3), lhsT=w("N"), rhs=identI, start=True, stop=True), [r("N"), Rc], [Rk])
                yield
                A(lambda e: e.activation(out=w("Nt"), in_=qv(bk, 3), func=AF.Copy), [Rk], [r("Nt")])
                yield
                V(lambda e: e.tensor_tensor(out=w("Qa"), in0=w("Nt"), in1=ident, op=ALU.add), [r("Nt"), Rc], [r("Qa")])
                cn, cnt_, qa, qb = "N", "Nt", "Qa", "Qb"
                for k in range(1, 7):
                    nn, nnt = ("N2", "Nt2") if cn == "N" else ("N", "Nt")
                    P(lambda e: e.matmul(qv(bk, 0), lhsT=w(cnt_), rhs=w(cn), start=True, stop=True), [r(cnt_), r(cn)], [Rk])
                    if k < 6:
                        P(lambda e: e.matmul(qv(bk, 1), lhsT=w(cn), rhs=w(cnt_), start=True, stop=True), [r(cnt_), r(cn)], [Rk])
                    yield
                    A(lambda e: e.activation(out=w(nn), in_=qv(bk, 0), func=AF.Copy), [Rk], [r(nn)])
                    if k < 6:
                        V(lambda e: e.tensor_copy(out=w(nnt), in_=qv(bk, 1)), [Rk], [r(nnt)])
                    yield
                    P(lambda e: e.matmul(qv(bk, 2), lhsT=w(nn), rhs=w(qa), start=True, stop=True), [r(nn), r(qa)], [Rk])
                    yield
                    if k < 6:
                        V(lambda e: e.scalar_tensor_tensor(out=w(qb), in0=qv(bk, 2), scalar=1.0, in1=w(qa), op0=ALU.mult, op1=ALU.add), [Rk, r(qa)], [r(qb)])
                    else:
                        V(lambda e: e.scalar_tensor_tensor(out=w("TT"), in0=qv(bk, 2), scalar=1.0, in1=w(qa), op0=ALU.mult, op1=ALU.add), [Rk, r(qa)], [r("TT")])
                    cn, cnt_ = nn, nnt
                    qa, qb = qb, qa
                yield
                P(lambda e: e.matmul(qv(bk, 3), lhsT=w("TT"), rhs=w("vb"), start=True, stop=True), [r("TT"), r("vb")], [Rk])
                P(lambda e: e.matmul(qv(bk, 1), lhsT=w("kbg"), rhs=w("TT"), start=True, stop=True), [r("TT"), r("kbg")], [Rk])
                yield
                A(lambda e: e.activation(out=w("u"), in_=qv(bk, 3), func=AF.Copy), [Rk], [r("u")])
                A(lambda e: e.activation(out=w("wT"), in_=qv(bk, 1), func=AF.Copy), [Rk], [r("wT")])

            def seq(n, d, sl_):
                w = lambda nm: (W[(nm, d, "h", sl_)] if nm in HO else W[(nm, d)])[:]
                r = lambda nm: RW[(nm, d, "h", sl_)] if nm in HO else RW[(nm, d)]
                gs = lambda nm: gsc[(nm, d)][:, n:n + 1]
                bS = 4 + d
                tk = slice(n * 128, (n + 1) * 128)
                P(lambda e: e.matmul(qv(bS, 0), lhsT=w("wT"), rhs=w("Sb"), start=True, stop=True), [r("wT"), r("Sb")], [Rq[bS][0]])
                if n >= 2:
                    P(lambda e: e.matmul(qv(bS, 2), lhsT=qT[:, tk], rhs=w("Sb"), start=True, stop=True), [RqT, r("Sb")], [Rq[bS][2]])
                yield
                V(lambda e: e.tensor_tensor(out=w("vnew"), in0=w("u"), in1=qv(bS, 0), op=ALU.subtract), [r("u"), Rq[bS][0]], [r("vnew")])
                if n >= 2:
                    A(lambda e: e.activation(out=w("otmp"), in_=qv(bS, 2), func=AF.Copy, scale=gs("eg")), [Rq[bS][2], Rgs], [r("otmp")])
                yield
                P(lambda e: e.matmul(qv(bS, 1), lhsT=w("kg"), rhs=w("vnew"), start=True, stop=True), [r("kg"), r("vnew")], [Rq[bS][1]])
                if n >= 2:
                    P(lambda e: e.matmul(qv(bS, 3), lhsT=w("attnT"), rhs=w("vnew"), start=True, stop=True), [r("attnT"), r("vnew")], [Rq[bS][3]])
                yield
                V(lambda e: e.scalar_tensor_tensor(out=w("S"), in0=w("S"), scalar=gsc[("egl", d)][:, n:n + 1], in1=qv(bS, 1), op0=ALU.mult, op1=ALU.add),
                  [r("S"), Rgs, Rq[bS][1]], [r("S")])
                yield
                A(lambda e: e.activation(out=w("Sb"), in_=w("S"), func=AF.Copy), [r("S")], [r("Sb")])
                if n >= 2:
                    lt = n - 2
                    if lt not in touched:
                        touched.add(lt)
                        V(lambda e: e.tensor_tensor(out=oacc[:, lt, :], in0=w("otmp"), in1=qv(bS, 3), op=ALU.add), [r("otmp"), Rq[bS][3]], [Roacc[lt]])
                    else:
                        V(lambda e: e.tensor_tensor(out=w("otmp2"), in0=w("otmp"), in1=qv(bS, 3), op=ALU.add), [r("otmp"), Rq[bS][3]], [r("otmp2")])
                        yield
                        G(lambda e: e.tensor_tensor(out=oacc[:, lt, :], in0=oacc[:, lt, :], in1=w("otmp2"), op=ALU.add), [r("otmp2"), Roacc[lt]], [Roacc[lt]])

            order = [[0, 1] + list(range(2, NT)), [1, 0] + list(range(NT - 1, 1, -1))]
            import os as _os
            nsteps = NT if stage >= 3 else int(_os.environ.get('KNSTEPS', '6'))
            nprep = [0, 0]; prep_act = [[], []]; prep_done = [set(), set()]
            nseq = [0, 0]; seq_act = [None, None]
            while nseq[0] < nsteps or nseq[1] < nsteps:
                for d in range(2):
                    while (len(prep_act[d]) < NCTX and nprep[d] < nsteps and nprep[d] < nseq[d] + NSLOT
                           and all(p_[0] % NCTX != nprep[d] % NCTX for p_ in prep_act[d])):
                        s_n = nprep[d]
                        prep_act[d].append((s_n, prep(order[d][s_n], d, s_n % NCTX, s_n % NSLOT)))
                        nprep[d] += 1
                    if seq_act[d] is None and nseq[d] < nsteps and nseq[d] in prep_done[d]:
                        seq_act[d] = seq(order[d][nseq[d]], d, nseq[d] % NSLOT)
                for d in range(2):
                    if seq_act[d] is not None:
                        try:
                            next(seq_act[d])
                        except StopIteration:
                            seq_act[d] = None
                            nseq[d] += 1
                for d in range(2):
                    for p_ in list(prep_act[d]):
                        try:
                            next(p_[1])
                        except StopIteration:
                            prep_act[d].remove(p_)
                            prep_done[d].add(p_[0])
            if dbg:
                Rd = Reg()
                d_ = dout("dbg_oacc", [128, 64 * 128])
                fw.dma("sp", lambda e: e.dma_start(out=d_, in_=oacc[:].rearrange("p t c -> p (t c)")), Roacc, [Rd])
                for d in range(2):
                    d_ = dout("dbg_S%d" % d, [128, 128])
                    fw.dma("sp", lambda e: e.dma_start(out=d_, in_=W[("S", d)][:]), [RW[("S", d)]], [Rd])
                    d_ = dout("dbg_TT%d" % d, [128, 128], BF16)
                    fw.dma("sp", lambda e: e.dma_start(out=d_, in_=W[("TT", d)][:]), [RW[("TT", d)]], [Rd])
                fw.finish([Rd])
            fw.pop_scope()
            fw.pop_scope()
        if stage >= 4:
            yloc_c = [nc.dram_tensor("yloc%d" % k, [256, 2048], BF16).ap() for k in range(4)]
            ycat_d = nc.dram_tensor("ycat", [4 * 1024, 2048], BF16).ap()
            Ryloc = [Reg() for _ in range(4)]; Rycat = Reg()
            fw.push_scope()
            wsf = sb("wsf", [128, 128]); Rwsf = Reg()
            fw.dma("sp", lambda e: e.dma_start(out=wsf[:], in_=gmws_d), writes=[Rwsf])
            wsT = sb("wsT", [128, 128], BF16); RwsT = Reg()
            P(lambda e: e.matmul(bank[0][:, 0:128], lhsT=wsf[:], rhs=ident, start=True, stop=True), [Rwsf, Rc], [Rb[0]])
            V(lambda e: e.tensor_copy(out=wsT[:], in_=bank[0][:, 0:128]), [Rb[0]], [RwsT])
            yst = [sb("yst%d" % i, [128, 2, 512], BF16) for i in range(2)]; Ryst = [Reg(), Reg()]
            junk5 = sb("junk5", [128, 128]); Rj5 = Reg()
            st5 = [sb("st5_%d" % i, [128, 4]) for i in range(2)]; Rst5 = [Reg(), Reg()]
            t1 = [sb("t1_%d" % i, [128, 128]) for i in range(2)]; Rt1 = [Reg(), Reg()]
            ybt = [sb("ybt%d" % i, [128, 128], BF16) for i in range(2)]; Rybt = [Reg(), Reg()]
            t2 = [sb("t2_%d" % i, [128, 128]) for i in range(2)]; Rt2 = [Reg(), Reg()]
            for lt in range(64):
                s_ = lt % 2
                blk = lt // 4
                ys = yst[blk % 2]; Rys = Ryst[blk % 2]
                cs = slice((lt % 4) * 128, (lt % 4 + 1) * 128)
                A(lambda e: e.activation(out=junk5[:], in_=oacc[:, lt, :], func=AF.Square, accum_out=st5[s_][:, 0:1]), [Roacc[lt]], [Rj5, Rst5[s_]])
                A(lambda e: e.activation(out=st5[s_][:, 1:2], in_=st5[s_][:, 0:1], func=AF.Sqrt, scale=1.0 / 128, bias=epst[:, 0:1]), [Rst5[s_], Rc], [Rst5[s_]])
                V(lambda e: e.reciprocal(out=st5[s_][:, 2:3], in_=st5[s_][:, 1:2]), [Rst5[s_]], [Rst5[s_]])
                V(lambda e: e.scalar_tensor_tensor(out=t1[s_][:], in0=oacc[:, lt, :], scalar=st5[s_][:, 2:3], in1=srow[:, 4:132], op0=ALU.mult, op1=ALU.mult),
                  [Roacc[lt], Rst5[s_], Rsrow], [Rt1[s_]])
                V(lambda e: e.tensor_tensor(out=ybt[s_][:], in0=t1[s_][:], in1=zs[:, lt, :], op=ALU.mult), [Rt1[s_], Rzs], [Rybt[s_]])
                bkT = 1 + s_
                pTb = bank[bkT][:, :].bitcast(BF16)
                P(lambda e: e.transpose(out=pTb[:, 0:128], in_=ybt[s_][:], identity=identb[:]), [Rybt[s_], Rc], [Rb[bkT]])
                A(lambda e: e.activation(out=ys[:, 1, cs], in_=pTb[:, 0:128], func=AF.Copy), [Rb[bkT]], [Rys])
                bkS = 3 + s_
                P(lambda e: e.matmul(bank[bkS][:, 0:128], lhsT=vgn[:, lt, :], rhs=wsT[:], start=True, stop=True), [Rvgn, RwsT], [Rb[bkS]])
                V(lambda e: e.tensor_tensor(out=t2[s_][:], in0=bank[bkS][:, 0:128], in1=srow[:, 260:388], op=ALU.add), [Rb[bkS], Rsrow], [Rt2[s_]])
                V(lambda e: e.tensor_tensor(out=ys[:, 0, cs], in0=t2[s_][:], in1=uT[:, lt * 128:(lt + 1) * 128], op=ALU.mult), [Rt2[s_], RuT], [Rys])
                if lt % 4 == 3:
                    ck = blk // 4
                    c0 = (blk % 4) * 512
                    fw.dma("sp", lambda e: e.dma_start(out=yloc_c[ck][:, c0:c0 + 512].rearrange("(a p) n -> p a n", p=128), in_=ys[:]), [Rys], [Ryloc[ck]])
                    if blk % 4 == 3:
                        fw.cc(lambda e: e.collective_compute("AllGather", ALU.bypass, replica_groups=[[0, 1, 2, 3], [4, 5, 6, 7]],
                                                             ins=[yloc_c[ck]], outs=[ycat_d[ck * 1024:(ck + 1) * 1024, :]]), [Ryloc[ck]], [Rycat])
            fw.pop_scope()
            if dbg:
                Rd = Reg()
                for ck in range(4):
                    d_ = dout("dbg_yloc%d" % ck, [256, 2048], BF16)
                    fw.dma("sp", lambda e: e.dma_start(out=d_, in_=yloc_c[ck]), [Ryloc[ck]], [Rd])
                d_ = dout("dbg_ycat", [4096, 2048], BF16)
                fw.dma("sp", lambda e: e.dma_start(out=d_, in_=ycat_d), [Rycat], [Rd])
                fw.finish([Rd])
        fw.pop_scope()
        if stage >= 5:
            h_d = nc.dram_tensor("h_scr", [2048, D], F32).ap()
            fin_d = nc.dram_tensor("fin_scr", [2048 + 128, D], BF16).ap()
            affloc_d = nc.dram_tensor("affloc", [128, 256], F32).ap()
            affall_d = nc.dram_tensor("affall", [512, 256], F32).ap()
            Rh = Reg(); Rfin = Reg(); Raffloc = Reg(); Raffall = Reg()
            mrow = sb("mrow", [128, 6 * D]); Rmrow = Reg()
            fw.dma("sp", lambda e: e.dma_start(out=mrow[:], in_=modrow_d), [Rmodrow], [Rmrow])
            affsb = sb("affsb", [128, 16, NE]); Raffsb = Reg()
            fw.push_scope()
            yidx = sb("yidx", [128, 8], I32); Ryidx = Reg()
            fw.dma("sp", lambda e: e.dma_start(out=yidx[:], in_=yidx_d), writes=[Ryidx])
            wo = sb("wo", [128, 8, D], BF16); Rwo = Reg()
            fw.dma("pool", lambda e: e.dma_start(out=wo[:], in_=wout_d.rearrange("(j k) n -> k j n", k=128)), writes=[Rwo])
            wr = sb("wr", [128, 8, NE]); Rwr = Reg()
            fw.dma("sp", lambda e: e.dma_start(out=wr[:], in_=wr_d.rearrange("(j k) n -> k j n", k=128)), writes=[Rwr])
            brow = sb("brow", [1, NE]); Rbrow = Reg()
            fw.dma("sp", lambda e: e.dma_start(out=brow[:], in_=br_d), writes=[Rbrow])
            ysb = sb("ysb", [128, 8, 2048], BF16); Rysb = [Reg() for _ in range(8)]
            for j in range(8):
                fw.dma("pool", lambda e: e.indirect_dma_start(out=ysb[:, j, :], out_offset=None, in_=ycat_d[:, :],
                                                              in_offset=bass.IndirectOffsetOnAxis(ap=yidx[:, j:j + 1], axis=0)),
                       [Rycat, Ryidx], [Rysb[j]])
            xo = [sb("xo%d" % i, [128, D]) for i in range(2)]; Rxo = [Reg(), Reg()]
            hsb = [sb("hsb%d" % i, [128, D]) for i in range(2)]; Rhsb = [Reg(), Reg()]
            fsb = [sb("fsb%d" % i, [128, D]) for i in range(2)]; Rfsb = [Reg(), Reg()]
            fbf = [sb("fbf%d" % i, [128, D], BF16) for i in range(2)]; Rfbf = [Reg(), Reg()]
            fT = [sb("fT%d" % i, [128, 8, 128]) for i in range(2)]; RfT = [Reg(), Reg()]
            jb = sb("jb", [128, D]); Rjb = Reg()
            stb = [sb("stb%d" % i, [128, 8]) for i in range(2)]; Rstb = [Reg(), Reg()]
            lg = [sb("lg%d" % i, [128, NE]) for i in range(2)]; Rlg = [Reg(), Reg()]
            def b_tile(ti):
                s_ = ti % 2
                b0, b1 = (0, 1) if s_ == 0 else (2, 3)
                fw.dma("sp", lambda e: e.dma_start(out=xo[s_][:], in_=xown_d[ti * 128:(ti + 1) * 128, :]), writes=[Rxo[s_]])
                for hf, bk in ((0, b0), (1, b1)):
                    for j in range(8):
                        P(lambda e: e.matmul(bank[bk][:, :], lhsT=ysb[:, j, ti * 128:(ti + 1) * 128], rhs=wo[:, j, hf * 512:(hf + 1) * 512],
                                             start=(j == 0), stop=(j == 7)), [Rysb[j], Rwo], [Rb[bk]])
                yield
                for hf, bk in ((0, b0), (1, b1)):
                    cs = slice(hf * 512, (hf + 1) * 512)
                    V(lambda e: e.tensor_tensor(out=hsb[s_][:, cs], in0=bank[bk][:, :], in1=mrow[:, hf * 512:(hf + 1) * 512], op=ALU.mult),
                      [Rb[bk], Rmrow], [Rhsb[s_]])
                yield
                V(lambda e: e.tensor_tensor(out=hsb[s_][:], in0=hsb[s_][:], in1=xo[s_][:], op=ALU.add), [Rhsb[s_], Rxo[s_]], [Rhsb[s_]])
                yield
                fw.dma("sp", lambda e: e.dma_start(out=h_d[ti * 128:(ti + 1) * 128, :], in_=hsb[s_][:]), [Rhsb[s_]], [Rh])
                A(lambda e: e.activation(out=jb[:], in_=hsb[s_][:], func=AF.Square, accum_out=stb[s_][:, 0:1]), [Rhsb[s_]], [Rjb, Rstb[s_]])
                yield
                A(lambda e: e.activation(out=stb[s_][:, 1:2], in_=stb[s_][:, 0:1], func=AF.Sqrt, scale=1.0 / D, bias=epst[:, 0:1]), [Rstb[s_], Rc], [Rstb[s_]])
                yield
                V(lambda e: e.reciprocal(out=stb[s_][:, 2:3], in_=stb[s_][:, 1:2]), [Rstb[s_]], [Rstb[s_]])
                yield
                V(lambda e: e.scalar_tensor_tensor(out=fsb[s_][:], in0=hsb[s_][:], scalar=stb[s_][:, 2:3], in1=mrow[:, 2 * D:3 * D], op0=ALU.mult, op1=ALU.mult),
                  [Rhsb[s_], Rstb[s_], Rmrow], [Rfsb[s_]])
                yield
                V(lambda e: e.tensor_tensor(out=fsb[s_][:], in0=fsb[s_][:], in1=mrow[:, D:2 * D], op=ALU.add), [Rfsb[s_], Rmrow], [Rfsb[s_]])
                yield
                A(lambda e: e.activation(out=fbf[s_][:], in_=fsb[s_][:], func=AF.Copy), [Rfsb[s_]], [Rfbf[s_]])
                for j in range(8):
                    bk = b0 if j < 4 else b1
                    q_ = j % 4
                    P(lambda e: e.matmul(bank[bk][:, q_ * 128:(q_ + 1) * 128], lhsT=fsb[s_][:, j * 128:(j + 1) * 128], rhs=ident, start=True, stop=True),
                      [Rfsb[s_], Rc], [Rb[bk]])
                yield
                fw.dma("sp", lambda e: e.dma_start(out=fin_d[ti * 128:(ti + 1) * 128, :], in_=fbf[s_][:]), [Rfbf[s_]], [Rfin])
                V(lambda e: e.tensor_copy(out=fT[s_][:, 0:4, :], in_=bank[b0][:, :].rearrange("p (q c) -> p q c", c=128)), [Rb[b0]], [RfT[s_]])
                A(lambda e: e.activation(out=fT[s_][:, 4:8, :], in_=bank[b1][:, :].rearrange("p (q c) -> p q c", c=128), func=AF.Copy), [Rb[b1]], [RfT[s_]])
                yield
                for j in range(8):
                    P(lambda e: e.matmul(bank[b0][:, 0:NE], lhsT=fT[s_][:, j, :], rhs=wr[:, j, :], start=(j == 0), stop=False), [RfT[s_], Rwr], [Rb[b0]])
                P(lambda e: e.matmul(bank[b0][:, 0:NE], lhsT=ones[0:1, :], rhs=brow[0:1, :], start=False, stop=True), [Rc, Rbrow], [Rb[b0]])
                yield
                V(lambda e: e.tensor_copy(out=lg[s_][:], in_=bank[b0][:, 0:NE]), [Rb[b0]], [Rlg[s_]])
                yield
                V(lambda e: e.tensor_reduce(out=stb[s_][:, 3:4], in_=lg[s_][:], axis=AX.X, op=ALU.max), [Rlg[s_]], [Rstb[s_]])
                yield
                V(lambda e: e.tensor_scalar(out=stb[s_][:, 4:5], in0=stb[s_][:, 3:4], scalar1=-1.0, scalar2=None, op0=ALU.mult), [Rstb[s_]], [Rstb[s_]])
                yield
                A(lambda e: e.activation(out=lg[s_][:], in_=lg[s_][:], func=AF.Exp, bias=stb[s_][:, 4:5], accum_out=stb[s_][:, 5:6]), [Rlg[s_], Rstb[s_]], [Rlg[s_], Rstb[s_]])
                yield
                V(lambda e: e.reciprocal(out=stb[s_][:, 6:7], in_=stb[s_][:, 5:6]), [Rstb[s_]], [Rstb[s_]])
                yield
                V(lambda e: e.tensor_scalar(out=affsb[:, ti, :], in0=lg[s_][:], scalar1=stb[s_][:, 6:7], scalar2=None, op0=ALU.mult), [Rlg[s_], Rstb[s_]], [Raffsb])
            for t2 in range(0, 16, 2):
                run_rr([b_tile(t2), b_tile(t2 + 1)])
            fw.dma("sp", lambda e: e.dma_start(out=affloc_d, in_=affsb[:].rearrange("p t e -> p (t e)")), [Raffsb], [Raffloc])
            fw.pop_scope()
            fw.cc(lambda e: e.collective_compute("AllGather", ALU.bypass, replica_groups=[[0, 1, 2, 3], [4, 5, 6, 7]],
                                                 ins=[affloc_d], outs=[affall_d]), [Raffloc], [Raffall])
            if dbg:
                Rd = Reg()
                d_ = dout("dbg_h", [2048, D])
                fw.dma("sp", lambda e: e.dma_start(out=d_, in_=h_d), [Rh], [Rd])
                d_ = dout("dbg_fin", [2048, D], BF16)
                fw.dma("sp", lambda e: e.dma_start(out=d_, in_=fin_d[0:2048, :]), [Rfin], [Rd])
                d_ = dout("dbg_affall", [512, 256])
                fw.dma("sp", lambda e: e.dma_start(out=d_, in_=affall_d), [Raffall], [Rd])
                fw.finish([Rd])
        if stage <= 5:
            Ro = Reg()
            fw.dma("sp", lambda e: e.dma_start(out=out_d[0:128, 0:32], in_=fm[:].rearrange("k a n -> k (a n)")), [Rfm], [Ro])
            fw.finish([Ro])
            return nc, list(dbg_d.keys())
        NIT = 30
        rc = sb("rc", [128, 16 + SLOTS]); Rrc = Reg()
        fw.dma("sp", lambda e: e.dma_start(out=rc[:], in_=rc_d), writes=[Rrc])
        idxi = sb("idxi", [128, NE, 4], I32); Ridx = Reg()
        gate = sb("gate", [128, NE, 4]); Rgate = Reg()
        wgs = [sb("wgs%d" % i, [128, 8, D], BF16) for i in range(2)]; Rwg = [Reg(), Reg()]
        wus = [sb("wus%d" % i, [128, 8, D], BF16) for i in range(2)]; Rwu = [Reg(), Reg()]
        wds = [sb("wds%d" % i, [128, 8, D], BF16) for i in range(2)]; Rwd = [Reg(), Reg()]
        def loads(ex):
            s_ = ex % 2
            fw.dma("pool", lambda e: e.dma_start(out=wgs[s_][:], in_=wg_d[ex].rearrange("(j k) n -> k j n", k=128)), [], [Rwg[s_]])
            fw.dma("pool", lambda e: e.dma_start(out=wus[s_][:], in_=wu_d[ex].rearrange("(j k) n -> k j n", k=128)), [], [Rwu[s_]])
            fw.dma("pool", lambda e: e.dma_start(out=wds[s_][:], in_=wd_d[ex].rearrange("(j k) n -> k j n", k=128)), [], [Rwd[s_]])

        loads(0)
        loads(1)
        fw.push_scope()
        Aall = sb("Aall", [128, 4, 256]); RAall = Reg()
        fw.dma("sp", lambda e: e.dma_start(out=Aall[:], in_=affall_d.rearrange("(r p) n -> p r n", p=128)), [Raffall], [RAall])
        lo = sb("lo", [128, NE]); hi = sb("hi", [128, NE]); mid = sb("mid", [128, NE]); Rth = Reg()
        cmpt = sb("cmpt", [128, 1024]); Rcmp = Reg()
        cntp = sb("cntp", [128, NE]); Rcnt = Reg()
        ge = sb("ge", [128, NE]); dl = sb("dl", [128, NE]); dh = sb("dh", [128, NE])
        V(lambda e: e.memset(lo[:], 0.0), [], [Rth])
        V(lambda e: e.memset(hi[:], 1.0), [], [Rth])
        Aview = Aall[:].rearrange("p r (t e) -> p (r t) e", e=NE)
        for it in range(NIT):
            hw_ = 0.5 ** (it + 1)
            V(lambda e: e.tensor_scalar(out=mid[:], in0=lo[:], scalar1=hw_, scalar2=None, op0=ALU.add), [Rth], [Rth])
            V(lambda e: e.tensor_tensor(out=cmpt[:].rearrange("p (n e) -> p n e", e=NE), in0=Aview,
                                        in1=mid[:].unsqueeze(1).to_broadcast([128, 64, NE]), op=ALU.is_ge), [RAall, Rth], [Rcmp])
            V(lambda e: e.tensor_reduce(out=cntp[:], in_=cmpt[:].rearrange("p (n e) -> p e n", e=NE), axis=AX.X, op=ALU.add), [Rcmp], [Rcnt])
            P(lambda e: e.matmul(bank[0][:, 0:NE], lhsT=ones, rhs=cntp[:], start=True, stop=True), [Rcnt, Rc], [Rb[0]])
            V(lambda e: e.tensor_scalar(out=ge[:], in0=bank[0][:, 0:NE], scalar1=CAP - 0.5, scalar2=None, op0=ALU.is_ge), [Rb[0]], [Rth])
            V(lambda e: e.scalar_tensor_tensor(out=lo[:], in0=ge[:], scalar=hw_, in1=lo[:], op0=ALU.mult, op1=ALU.add), [Rth], [Rth])
        sel = sb("sel", [128, 256]); Rsel = Reg()
        V(lambda e: e.tensor_tensor(out=sel[:].rearrange("p (t e) -> p t e", e=NE), in0=affsb[:], in1=lo[:].unsqueeze(1).to_broadcast([128, 16, NE]), op=ALU.is_ge),
          [Raffsb, Rth], [Rsel])
        P(lambda e: e.matmul(bank[1][:, 0:256], lhsT=U_st, rhs=sel[:], start=True, stop=True), [Rsel, Rc], [Rb[1]])
        P(lambda e: e.matmul(bank[2][:, 0:256], lhsT=ones, rhs=sel[:], start=True, stop=True), [Rsel, Rc], [Rb[2]])
        tot = sb("tot", [128, 16, NE]); pre = sb("pre", [128, 16, NE]); Rpre_ = Reg()
        V(lambda e: e.tensor_copy(out=tot[:].rearrange("p t e -> p (t e)"), in_=bank[2][:, 0:256]), [Rb[2]], [Rpre_])
        V(lambda e: e.memset(pre[:, 0, :], 0.0), [], [Rpre_])
        for ti in range(1, 16):
            V(lambda e: e.tensor_tensor(out=pre[:, ti, :], in0=pre[:, ti - 1, :], in1=tot[:, ti - 1, :], op=ALU.add), [Rpre_], [Rpre_])
        rank = sb("rank", [128, 256]); Rrank = Reg()
        V(lambda e: e.tensor_tensor(out=rank[:], in0=bank[1][:, 0:256], in1=pre[:].rearrange("p t e -> p (t e)"), op=ALU.add), [Rb[1], Rpre_], [Rrank])
        Rall = sb("Rall", [128, 16, NE, 4]); RRall = Reg()
        V(lambda e: e.memset(Rall[:].rearrange("p t e c -> p (t e c)"), 1.0), [], [RRall])
        V(lambda e: e.tensor_copy(out=Rall[:, :, :, 0], in_=rc[:, 0:16].unsqueeze(2).to_broadcast([128, 16, NE])), [Rrc], [RRall])
        V(lambda e: e.tensor_copy(out=Rall[:, :, :, 1], in_=affsb[:]), [Raffsb], [RRall])
        oh = [sb("oh%d" % i, [128, 16, SLOTS]) for i in range(2)]; Roh = [Reg(), Reg()]
        sinfo = sb("sinfo", [128, 3, 4]); Rsinfo = Reg()
        itmp = sb("itmp", [128, 3]); Ritmp = Reg()
        V(lambda e: e.memset(gate[:].rearrange("p e c -> p (e c)"), 0.0), [], [Rgate])
        def oh_ops(ex):
            o_ = ex % 2
            for ti in range(16):
                col = ti * NE + ex
                V(lambda e: e.tensor_scalar(out=oh[o_][:, ti, :], in0=rc[:, 16:16 + SLOTS], scalar1=rank[:, col:col + 1], scalar2=sel[:, col:col + 1],
                                            op0=ALU.is_equal, op1=ALU.mult), [Rrc, Rrank, Rsel], [Roh[o_]])

        def mm_ops(ex):
            bkI = 3 + (ex % 2)
            o_ = ex % 2
            for c in range(3):
                M = 128 if c < 2 else SLOTS - 256
                for ti in range(16):
                    P(lambda e: e.matmul(bank[bkI][0:M, c * 128:c * 128 + 4], lhsT=oh[o_][:, ti, c * 128:c * 128 + M], rhs=Rall[:, ti, ex, :],
                                         start=(ti == 0), stop=(ti == 15)), [Roh[o_], RRall], [Rb[bkI]])

        def sinfo_ops(ex):
            bkI = 3 + (ex % 2)
            q_ = ex % 2
            V(lambda e: e.memset(sinfo2[q_][:].rearrange("p c k -> p (c k)"), 0.0), [], [Rsinfo2[q_]])
            V(lambda e: e.tensor_copy(out=sinfo2[q_][:, 0, :], in_=bank[bkI][:, 0:4]), [Rb[bkI]], [Rsinfo2[q_]])
            V(lambda e: e.tensor_copy(out=sinfo2[q_][:, 1, :], in_=bank[bkI][:, 128:132]), [Rb[bkI]], [Rsinfo2[q_]])
            V(lambda e: e.tensor_copy(out=sinfo2[q_][0:64, 2, :], in_=bank[bkI][0:64, 256:260]), [Rb[bkI]], [Rsinfo2[q_]])
            V(lambda e: e.tensor_scalar(out=itmp2[q_][:], in0=sinfo2[q_][:, :, 2], scalar1=-2048.0, scalar2=2048.0, op0=ALU.mult, op1=ALU.add), [Rsinfo2[q_]], [Ritmp2[q_]])
            V(lambda e: e.tensor_tensor(out=itmp2[q_][:], in0=itmp2[q_][:], in1=sinfo2[q_][:, :, 0], op=ALU.add), [Rsinfo2[q_], Ritmp2[q_]], [Ritmp2[q_]])
            V(lambda e: e.tensor_copy(out=idxi[:, ex, 0:3], in_=itmp2[q_][:]), [Ritmp2[q_]], [Ridx])
            V(lambda e: e.tensor_copy(out=gate[:, ex, 0:3], in_=sinfo2[q_][:, :, 1]), [Rsinfo2[q_]], [Rgate])

        sinfo2 = [sinfo, sb("sinfo_b", [128, 3, 4])]; Rsinfo2 = [Rsinfo, Reg()]
        itmp2 = [itmp, sb("itmp_b", [128, 3])]; Ritmp2 = [Ritmp, Reg()]
        oh_ops(0)
        for ex in range(NE):
            if ex + 1 < NE:
                oh_ops(ex + 1)
            mm_ops(ex)
            sinfo_ops(ex)
        if dbg:
            Rd = Reg()
            d_ = dout("dbg_lo", [128, NE])
            fw.dma("sp", lambda e: e.dma_start(out=d_, in_=lo[:]), [Rth], [Rd])
            d_ = dout("dbg_hi", [128, NE])
            fw.dma("sp", lambda e: e.dma_start(out=d_, in_=hi[:]), [Rth], [Rd])
            d_ = dout("dbg_rank", [128, 256])
            fw.dma("sp", lambda e: e.dma_start(out=d_, in_=rank[:]), [Rrank], [Rd])
            d_ = dout("dbg_sel", [128, 256])
            fw.dma("sp", lambda e: e.dma_start(out=d_, in_=sel[:]), [Rsel], [Rd])
            d_ = dout("dbg_idxi", [128, NE * 4], I32)
            fw.dma("sp", lambda e: e.dma_start(out=d_, in_=idxi[:].rearrange("p e c -> p (e c)")), [Ridx], [Rd])
            d_ = dout("dbg_sinfo", [128, 12])
            fw.dma("sp", lambda e: e.dma_start(out=d_, in_=sinfo[:].rearrange("p c k -> p (c k)")), [Rsinfo], [Rd])
            d_ = dout("dbg_Rall", [128, 16 * NE * 4])
            fw.dma("sp", lambda e: e.dma_start(out=d_, in_=Rall[:].rearrange("p t e c -> p (t e c)")), [RRall], [Rd])
            d_ = dout("dbg_gate", [128, NE * 4])
            fw.dma("sp", lambda e: e.dma_start(out=d_, in_=gate[:].rearrange("p e c -> p (e c)")), [Rgate], [Rd])
            fw.finish([Rd])
        fw.pop_scope()
        if stage <= 6:
            Ro = Reg()
            fw.dma("sp", lambda e: e.dma_start(out=out_d[0:128, 0:32], in_=fm[:].rearrange("k a n -> k (a n)")), [Rfm], [Ro])
            fw.finish([Ro])
            return nc, list(dbg_d.keys())
        acc_d = nc.dram_tensor("acc_scr", [2048 + 128, D], F32).ap()
        Racc = Reg()
        fw.push_scope()
        xe = [sb("xe%d" % i, [128, 3, D], BF16) for i in range(2)]; Rxe = [Reg(), Reg()]
        xeT = [sb("xeT%d" % i, [128, 8, SLOTS], BF16) for i in range(2)]; RxeT = [Reg(), Reg()]
        hid = [sb("hid%d" % i, [128, 8, SLOTS], BF16) for i in range(2)]; Rhid = [Reg(), Reg()]
        sg = [sb("sg%d" % i, [128, SLOTS]) for i in range(2)]; Rsg = [Reg(), Reg()]
        ye0_ = sb("ye0", [128, 3, D]); Rye0_ = Reg()
        ye = [ye0_, ye0_]; Rye = [Rye0_, Rye0_]
        wdf = sb("wdf", [128, 8, D]); Rwdf = Reg()

        def loads_gu(ex):
            s_ = ex % 2
            fw.dma("pool", lambda e: e.dma_start(out=wgs[s_][:], in_=wg_d[ex].rearrange("(j k) n -> k j n", k=128)), [], [Rwg[s_]])
            fw.dma("pool", lambda e: e.dma_start(out=wus[s_][:], in_=wu_d[ex].rearrange("(j k) n -> k j n", k=128)), [], [Rwu[s_]])

        def dma_down(ex):
            fw.dma("sp", lambda e: e.dma_start(out=wdf[:], in_=wd_d[ex].rearrange("(j k) n -> k j n", k=128)), [], [Rwdf])

        def cast_down(ex):
            s_ = ex % 2
            A(lambda e: e.activation(out=wds[s_][:, 0:4, :], in_=wdf[:, 0:4, :], func=AF.Copy), [Rwdf], [Rwd[s_]])
            V(lambda e: e.tensor_copy(out=wds[s_][:, 4:8, :], in_=wdf[:, 4:8, :]), [Rwdf], [Rwd[s_]])

        for i in range(2):
            V(lambda e: e.memset(xe[i][:].rearrange("p c n -> p (c n)"), 0.0), [], [Rxe[i]])
        V(lambda e: e.memset(ye[0][:].rearrange("p c n -> p (c n)"), 0.0), [], [Rye[0]])
        for ti in range(17):
            fw.dma("sp", lambda e: e.dma_start(out=acc_d[ti * 128:(ti + 1) * 128, :], in_=ye[0][:, 0, :]), [Rye[0]], [Racc])
        fw.dma("sp", lambda e: e.dma_start(out=fin_d[2048:2176, :], in_=xe[0][:, 0, :]), [Rxe[0]], [Rfin])
        CH = [(0, 128), (1, 128), (2, SLOTS - 256)]

        def gather(ex):
            s_ = ex % 2
            for c, M in CH:
                fw.dma("pool", lambda e: e.indirect_dma_start(out=xe[s_][0:M, c, :], out_offset=None, in_=fin_d[:, :],
                                                              in_offset=bass.IndirectOffsetOnAxis(ap=idxi[0:M, ex, c:c + 1], axis=0)),
                       [Rfin, Ridx], [Rxe[s_]])

        def compute(ex):
            s_ = ex % 2
            for c, M in CH:
                bk = c % 2
                pTb = bank[bk][:, :].bitcast(BF16)
                for j in range(8):
                    P(lambda e: e.transpose(out=pTb[:, j * 128:j * 128 + M], in_=xe[s_][0:M, c, j * 128:(j + 1) * 128], identity=identb[0:M, 0:M]),
                      [Rxe[s_], Rc], [Rb[bk]])
                src = pTb.rearrange("p (j m) -> p j m", m=128)[:, :, 0:M]
                if c % 2 == 0:
                    V(lambda e: e.tensor_copy(out=xeT[s_][:, :, c * 128:c * 128 + M], in_=src), [Rb[bk]], [RxeT[s_]])
                else:
                    A(lambda e: e.activation(out=xeT[s_][:, :, c * 128:c * 128 + M], in_=src, func=AF.Copy), [Rb[bk]], [RxeT[s_]])
            for f in range(8):
                bg = 2 + (f % 2); bu = 4 + (f % 2); q_ = f % 2
                for j in range(8):
                    P(lambda e: e.matmul(bank[bg][:, 0:SLOTS], lhsT=wgs[s_][:, j, f * 128:(f + 1) * 128], rhs=xeT[s_][:, j, :], start=(j == 0), stop=(j == 7)),
                      [Rwg[s_], RxeT[s_]], [Rb[bg]])
                for j in range(8):
                    P(lambda e: e.matmul(bank[bu][:, 0:SLOTS], lhsT=wus[s_][:, j, f * 128:(f + 1) * 128], rhs=xeT[s_][:, j, :], start=(j == 0), stop=(j == 7)),
                      [Rwu[s_], RxeT[s_]], [Rb[bu]])
                A(lambda e: e.activation(out=sg[q_][:], in_=bank[bg][:, 0:SLOTS], func=AF.Silu), [Rb[bg]], [Rsg[q_]])
                V(lambda e: e.tensor_tensor(out=hid[s_][:, f, :], in0=bank[bu][:, 0:SLOTS], in1=sg[q_][:], op=ALU.mult), [Rb[bu], Rsg[q_]], [Rhid[s_]])
            for c, M in CH:
                for hf in range(2):
                    bd = 6 + ((c * 2 + hf) % 2)
                    for f in range(8):
                        P(lambda e: e.matmul(bank[bd][0:M, :], lhsT=hid[s_][:, f, c * 128:c * 128 + M], rhs=wds[s_][:, f, hf * 512:(hf + 1) * 512],
                                             start=(f == 0), stop=(f == 7)), [Rhid[s_], Rwd[s_]], [Rb[bd]])
                    if hf == 0:
                        V(lambda e: e.tensor_scalar(out=ye[s_][0:M, c, 0:512], in0=bank[bd][0:M, :], scalar1=gate[0:M, ex, c:c + 1], scalar2=None, op0=ALU.mult),
                          [Rb[bd], Rgate], [Rye[s_]])
                    else:
                        A(lambda e: e.activation(out=ye[s_][0:M, c, 512:1024], in_=bank[bd][0:M, :], func=AF.Copy, scale=gate[0:M, ex, c:c + 1]),
                          [Rb[bd], Rgate], [Rye[s_]])

        def scatter(ex):
            s_ = ex % 2
            for c, M in CH:
                fw.dma("pool", lambda e: e.indirect_dma_start(out=acc_d[:, :], out_offset=bass.IndirectOffsetOnAxis(ap=idxi[0:M, ex, c:c + 1], axis=0),
                                                              in_=ye[s_][0:M, c, :], in_offset=None,
                                                              compute_op=ALU.add),
                       [Rye[s_], Ridx, Racc], [Racc])

        nexp = NE if stage >= 8 else 2
        gather(0)
        for ex in range(nexp):
            if ex + 1 < nexp:
                if ex + 1 >= 2:
                    loads_gu(ex + 1)
                    dma_down(ex + 1)
                gather(ex + 1)
            compute(ex)
            if 2 <= ex + 1 < nexp:
                cast_down(ex + 1)
            scatter(ex)
        fw.pop_scope()
        fw.push_scope()
        at = [sb("at%d" % i, [128, D]) for i in range(4)]; Rat = [Reg() for _ in range(4)]
        ht = [sb("ht%d" % i, [128, D]) for i in range(4)]; Rht = [Reg() for _ in range(4)]
        ot = [sb("ot%d" % i, [128, D]) for i in range(4)]; Rot = [Reg() for _ in range(4)]
        jf = sb("jf", [128, D]); Rjf = Reg()
        stf = [sb("stf%d" % i, [128, 4]) for i in range(4)]; Rstf = [Reg() for _ in range(4)]
        Rout = Reg()
        def fin_tile(ti):
            s_ = ti % 4
            rs = slice(ti * 128, (ti + 1) * 128)
            fw.dma("sp", lambda e: e.dma_start(out=at[s_][:], in_=acc_d[rs, :]), [Racc], [Rat[s_]])
            fw.dma("sp", lambda e: e.dma_start(out=ht[s_][:], in_=h_d[rs, :]), [Rh], [Rht[s_]])
            yield
            V(lambda e: e.tensor_tensor(out=at[s_][:], in0=at[s_][:], in1=mrow[:, 3 * D:4 * D], op=ALU.mult), [Rat[s_], Rmrow], [Rat[s_]])
            yield
            V(lambda e: e.tensor_tensor(out=at[s_][:], in0=at[s_][:], in1=ht[s_][:], op=ALU.add), [Rat[s_], Rht[s_]], [Rat[s_]])
            yield
            A(lambda e: e.activation(out=jf[:], in_=at[s_][:], func=AF.Square, accum_out=stf[s_][:, 0:1]), [Rat[s_]], [Rjf, Rstf[s_]])
            yield
            A(lambda e: e.activation(out=stf[s_][:, 1:2], in_=stf[s_][:, 0:1], func=AF.Sqrt, scale=1.0 / D, bias=epst[:, 0:1]), [Rstf[s_], Rc], [Rstf[s_]])
            yield
            V(lambda e: e.reciprocal(out=stf[s_][:, 2:3], in_=stf[s_][:, 1:2]), [Rstf[s_]], [Rstf[s_]])
            yield
            V(lambda e: e.scalar_tensor_tensor(out=ot[s_][:], in0=at[s_][:], scalar=stf[s_][:, 2:3], in1=mrow[:, 5 * D:6 * D], op0=ALU.mult, op1=ALU.mult),
              [Rat[s_], Rstf[s_], Rmrow], [Rot[s_]])
            yield
            fw.dma("sp", lambda e: e.dma_start(out=out_d[rs, :], in_=ot[s_][:]), [Rot[s_]], [Rout])
        for t2 in range(0, 16, 4):
            run_rr([fin_tile(t2 + i) for i in range(4)])
        if dbg:
            Rd = Reg()
            d_ = dout("dbg_acc", [2048, D])
            fw.dma("sp", lambda e: e.dma_start(out=d_, in_=acc_d[0:2048, :]), [Racc], [Rd])
        fw.finish([Rout])
        fw.pop_scope()
        return nc, list(dbg_d.keys())
        if stage <= 4:
            Ro = Reg()
            fw.dma("sp", lambda e: e.dma_start(out=out_d[0:128, 0:32], in_=fm[:].rearrange("k a n -> k (a n)")), [Rfm], [Ro])
            fw.finish([Ro])
            return nc, list(dbg_d.keys())
        if stage <= 1:
            Ro = Reg()
            fw.dma("sp", lambda e: e.dma_start(out=out_d[0:128, 0:32], in_=fm[:].rearrange("k a n -> k (a n)")), [Rfm], [Ro])
            fw.finish([Ro])
            return nc, list(dbg_d.keys())
    return nc, list(dbg_d.keys())


def make_inputs(inputs):
    f = lambda a: np.ascontiguousarray(np.asarray(a, dtype=np.float32))
    x = f(inputs["x"]); c = f(inputs["c"]); ctx = f(inputs["ctx"]); c_ctx = f(inputs["c_ctx"])
    w_mod = f(inputs["w_mod"])[0]; b_mod = f(inputs["b_mod"])[0]
    w_in = f(inputs["w_in"])[0]; conv_w = f(inputs["conv_w"])[0]
    a_log = f(inputs["a_log"])[0]; dt_bias = f(inputs["dt_bias"])[0]
    gdn_g = f(inputs["gdn_norm_g"])[0]; gm_g = f(inputs["gm_norm_g"])[0]
    gm_ws = f(inputs["gm_ws"])[0]; gm_bs = f(inputs["gm_bs"])[0]
    w_out = f(inputs["w_out"])[0]
    w_router = f(inputs["w_router"])[0]; b_router = f(inputs["b_router"])[0]
    w_gate = f(inputs["w_gate"])[0]; w_up = f(inputs["w_up"])[0]; w_down = f(inputs["w_down"])[0]
    rcst = np.concatenate([(np.arange(16)[None, :] * 128 + np.arange(128)[:, None]).astype(np.float32),
                           np.tile(np.arange(SLOTS, dtype=np.float32)[None, :], (128, 1))], axis=1)
    nrm = np.concatenate([f(inputs["norm1_g"])[0], f(inputs["norm2_g"])[0], f(inputs["final_norm_g"])])[None, :]
    r = np.arange(128)
    cst = np.concatenate([
        np.eye(128), (r[None, :] >= r[:, None]), (r[None, :] <= r[:, None]), (r[None, :] > r[:, None]),
        (r[None, :] < r[:, None]), np.ones((128, 128))], axis=1).astype(np.float32)
    maps = []
    for core in range(8):
        b, h = core // 4, core % 4
        cols = np.concatenate([
            np.arange(h * 128, (h + 1) * 128),
            512 + np.arange(h * 128, (h + 1) * 128),
            1024 + np.arange(h * 128, (h + 1) * 128),
            2064 + np.arange(h * 128, (h + 1) * 128),
            1552 + np.arange(h * 128, (h + 1) * 128),
            2576 + np.arange(h * 128, (h + 1) * 128),
            np.array([1536 + h, 1540 + h, 1544 + h, 1548 + h]),
        ])
        qkv_cols = np.concatenate([np.arange(h * 128, (h + 1) * 128), 512 + np.arange(h * 128, (h + 1) * 128),
                                   1024 + np.arange(h * 128, (h + 1) * 128)])
        small = np.concatenate([
            conv_w[:, qkv_cols].reshape(-1), a_log[:, h], dt_bias[:, h], gdn_g,
            gm_g[h * 128:(h + 1) * 128], gm_bs[h]])[None, :]
        perm = np.concatenate([np.concatenate([np.arange(j * 128, (j + 1) * 128), 512 + np.arange(j * 128, (j + 1) * 128)])
                               for j in range(4)])
        maps.append({
            "x": x[b], "ctx": ctx[b],
            "crow": np.concatenate([c[b], c_ctx])[None, :].copy(),
            "w_mod": w_mod, "b_mod": b_mod[None, :].copy(), "nrm": nrm.copy(),
            "w_in": np.ascontiguousarray(w_in[:, cols]),
            "small": np.ascontiguousarray(small.astype(np.float32)),
            "gm_ws": np.ascontiguousarray(gm_ws[h]),
            "w_out": np.ascontiguousarray(w_out[perm, :]),
            "cst": cst,
            "x_own": np.ascontiguousarray(x[b, h * 2048:(h + 1) * 2048]),
            "yidx": (h * 1024 + np.arange(8)[None, :] * 128 + np.arange(128)[:, None]).astype(np.int32),
            "w_router": w_router, "b_router": b_router[None, :].copy(),
            "w_gate": w_gate, "w_up": w_up, "w_down": w_down,
            "rcst": rcst,
        })
    return maps


def kernel(**inputs):
    maps = make_inputs(inputs)
    nc, _ = build_program(stage=99)
    res = run_bass_kernel_spmd(nc, maps, core_ids=list(range(8)))
    out = np.zeros((2, T, D), np.float32)
    for core in range(8):
        b, h = core // 4, core % 4
        out[b, h * 2048:(h + 1) * 2048] = res.results[core]["out"]
    return out
```
